# Optimizing a Trainium2 kernel written in Bass

```python
import math
import jax, jax.numpy as jnp
from jax import lax
import numpy as np


D_MODEL = 2048
BATCH = 2
SEQ = 16384
DEPTH = 2

N_A_LAYERS = DEPTH // 2
N_B_LAYERS = DEPTH - N_A_LAYERS
ALPHA = (2 * DEPTH) ** 0.25
BETA = (8 * DEPTH) ** -0.25
LN_EPS = 1e-5
RW_HEAD_DIM = 64
RW_HEADS = D_MODEL // RW_HEAD_DIM
RW_DECAY_RANK = 96
RW_A_RANK = 96
RW_GATE_RANK = 256
RW_GN_EPS = 64e-5
NSA_HEAD_DIM = 128
NSA_HEADS = D_MODEL // NSA_HEAD_DIM
NSA_KV_GROUPS = 2
NSA_HPG = NSA_HEADS // NSA_KV_GROUPS
CMP_STRIDE = 16
CMP_LEN = 2 * CMP_STRIDE
CMP_HIDDEN = 2 * NSA_HEAD_DIM
SEL_BLOCK = 64
N_SEL = 16
WINDOW = 512
Q_BLOCK = 128
N_EXPERTS = 32
TOP_K = 4
D_EXPERT = D_MODEL
SWIGLU_LIMIT = 7.0
SWIGLU_ALPHA = 1.702
ROW_BLOCK = 256

kernel_name = 'hybrid_rwkv7_nsa_yoco_moe'


def layer_norm(z, g, b):
    zf = z.astype(jnp.float32)
    mu = jnp.mean(zf, -1, keepdims=True)
    var = jnp.mean(jnp.square(zf - mu), -1, keepdims=True)
    return ((zf - mu) * lax.rsqrt(var + LN_EPS) * g + b).astype(z.dtype)


def masked_softmax(s, mask):
    s = jnp.where(mask, s, -jnp.inf)
    m = jnp.max(s, -1, keepdims=True)
    m = jnp.where(jnp.isfinite(m), m, 0.0)
    e = jnp.exp(s - m)
    d = jnp.sum(e, -1, keepdims=True)
    return e / jnp.where(d > 0, d, 1.0)


def wkv7_scan(r, w, k, v, a, b):
    B, T, H, N = r.shape

    def step(S, inp):
        r_t, w_t, k_t, v_t, a_t, b_t = inp
        sa = jnp.einsum('bhij,bhj->bhi', S, a_t)
        S = S * w_t[:, :, None, :] + sa[..., None] * b_t[:, :, None, :] + v_t[..., None] * k_t[:, :, None, :]
        return S, jnp.einsum('bhij,bhj->bhi', S, r_t)

    S0 = jnp.zeros((B, H, N, N), jnp.float32)
    seq = tuple(jnp.moveaxis(z, 1, 0) for z in (r, w, k, v, a, b))
    _, y = lax.scan(step, S0, seq, unroll=4)
    return jnp.moveaxis(y, 0, 1)


def rwkv7_time_mix(x, mu, w_rkv, w0, w1, w2, a0, a1, a2, g1, g2, k_k, k_a, r_k, lnx_g, lnx_b, w_o):
    B, T, D = x.shape
    H, N = RW_HEADS, RW_HEAD_DIM
    xx = jnp.pad(x[:, :-1], ((0, 0), (1, 0), (0, 0))) - x
    xr, xw, xk, xv, xa, xg = [x + xx * mu[i] for i in range(6)]
    r, k, v = jnp.einsum('pbtd,pde->pbte', jnp.stack([xr, xk, xv]), w_rkv)
    w_log = -jax.nn.softplus(-(w0 + jnp.tanh(xw @ w1) @ w2).astype(jnp.float32)) - 0.5
    decay = jnp.exp(-jnp.exp(w_log))
    a = jax.nn.sigmoid((a0 + (xa @ a1) @ a2).astype(jnp.float32))
    g = jax.nn.sigmoid(xg @ g1) @ g2
    heads = lambda z: z.reshape(B, T, H, N).astype(jnp.float32)
    r_h, k_h, v_h, a_h, w_h = heads(r), heads(k), heads(v), heads(a), heads(decay)
    kk = k_h * k_k.reshape(H, N)
    kk = kk / jnp.maximum(jnp.linalg.norm(kk, axis=-1, keepdims=True), 1e-12)
    k_h = k_h * (1.0 + (a_h - 1.0) * k_a.reshape(H, N))
    y = wkv7_scan(r_h, w_h, k_h, v_h, -kk, kk * a_h)
    ym = jnp.mean(y, -1, keepdims=True)
    yv = jnp.mean(jnp.square(y - ym), -1, keepdims=True)
    y = ((y - ym) * lax.rsqrt(yv + RW_GN_EPS)).reshape(B, T, D) * lnx_g + lnx_b
    bonus = jnp.sum(r_h * k_h * r_k, -1, keepdims=True) * v_h
    y = y + bonus.reshape(B, T, D)
    return (y * g).astype(x.dtype) @ w_o


def nsa_shared_kv(s, w_kv, cmp_pe, cmp_w1, cmp_b1, cmp_w2, cmp_b2):
    B, T, D = s.shape
    G, DK = NSA_KV_GROUPS, NSA_HEAD_DIM
    kv = (s @ w_kv).reshape(B, T, 6, G, DK)
    kc, vc, ks, vs, kw, vw = [kv[:, :, i] for i in range(6)]
    n_c = T // CMP_STRIDE - 1

    def compress(z, i):
        ch = z.reshape(B, T // CMP_STRIDE, CMP_STRIDE, G, DK)
        blk = jnp.concatenate([ch[:, :-1], ch[:, 1:]], axis=2)
        blk = blk + cmp_pe[i][:, None, :]
        flat = blk.transpose(0, 1, 3, 2, 4).reshape(B, n_c, G, CMP_LEN * DK)
        hdn = jax.nn.gelu(flat @ cmp_w1[i] + cmp_b1[i])
        return hdn @ cmp_w2[i] + cmp_b2[i]

    k_cmp, v_cmp = compress(kc, 0), compress(vc, 1)
    n_s = T // SEL_BLOCK
    k_sel = ks.reshape(B, n_s, SEL_BLOCK, G, DK).transpose(0, 3, 1, 2, 4)
    v_sel = vs.reshape(B, n_s, SEL_BLOCK, G, DK).transpose(0, 3, 1, 2, 4)
    k_win = jnp.pad(kw, ((0, 0), (WINDOW, 0), (0, 0), (0, 0)))
    v_win = jnp.pad(vw, ((0, 0), (WINDOW, 0), (0, 0), (0, 0)))
    return k_cmp, v_cmp, k_sel, v_sel, k_win, v_win


def nsa_attention(x, shared, w_in, b_gate, w_o):
    k_cmp, v_cmp, k_sel, v_sel, k_win, v_win = shared
    B, T, D = x.shape
    G, HPG, DK = NSA_KV_GROUPS, NSA_HPG, NSA_HEAD_DIM
    H = NSA_HEADS
    nqb = T // Q_BLOCK
    scale = NSA_HEAD_DIM ** -0.5
    proj = x @ w_in
    q = jnp.moveaxis(proj[..., :H * DK].reshape(B, nqb, Q_BLOCK, G, HPG, DK), 1, 0)
    gates = jax.nn.sigmoid((proj[..., H * DK:] + b_gate).astype(jnp.float32))
    gates = jnp.moveaxis(gates.reshape(B, nqb, Q_BLOCK, 3, G, HPG), 1, 0)
    n_c = k_cmp.shape[1]
    n_s = k_sel.shape[2]
    n_sel = min(N_SEL, n_s)
    c_end = jnp.arange(n_c) * CMP_STRIDE + CMP_LEN - 1
    c_lo = jnp.arange(n_c) * CMP_STRIDE
    s_lo = jnp.arange(n_s) * SEL_BLOCK
    overlap = ((c_lo[:, None] < s_lo[None, :] + SEL_BLOCK) & (c_lo[:, None] + CMP_LEN > s_lo[None, :])).astype(jnp.float32)
    sel_ids = jnp.arange(n_s)
    bi = jnp.arange(B)[:, None, None, None]
    gi = jnp.arange(G)[None, :, None, None]

    def block_fn(args):
        qb, qblk, gblk = args
        t = qb * Q_BLOCK + jnp.arange(Q_BLOCK)
        s_c = jnp.einsum('bqghd,bcgd->bghqc', qblk, k_cmp, preferred_element_type=jnp.float32) * scale
        p_c = masked_softmax(s_c, c_end[None, :] <= t[:, None])
        o_c = jnp.einsum('bghqc,bcgd->bqghd', p_c, v_cmp.astype(jnp.float32))
        imp = jnp.einsum('bghqc,cs->bgqs', p_c, overlap)
        valid = s_lo[None, :] <= t[:, None]
        cur = (t // SEL_BLOCK)[:, None]
        forced = (sel_ids[None, :] == 0) | (sel_ids[None, :] == cur) | (sel_ids[None, :] == cur - 1)
        score = jnp.where(valid, jnp.where(forced, jnp.inf, imp), -jnp.inf)
        idx = lax.top_k(score, n_sel)[1]
        kg = k_sel[bi, gi, idx]
        vg = v_sel[bi, gi, idx]
        kpos = idx[..., None] * SEL_BLOCK + jnp.arange(SEL_BLOCK)
        mask_s = (kpos <= t[None, None, :, None, None]).reshape(B, G, 1, Q_BLOCK, n_sel * SEL_BLOCK)
        s_s = jnp.einsum('bqghd,bgqskd->bghqsk', qblk, kg, preferred_element_type=jnp.float32) * scale
        p_s = masked_softmax(s_s.reshape(B, G, HPG, Q_BLOCK, n_sel * SEL_BLOCK), mask_s)
        o_s = jnp.einsum('bghqk,bgqkd->bqghd', p_s, vg.reshape(B, G, Q_BLOCK, n_sel * SEL_BLOCK, DK).astype(jnp.float32))
        kw = lax.dynamic_slice_in_dim(k_win, qb * Q_BLOCK, WINDOW + Q_BLOCK, axis=1)
        vw = lax.dynamic_slice_in_dim(v_win, qb * Q_BLOCK, WINDOW + Q_BLOCK, axis=1)
        kpos_w = qb * Q_BLOCK - WINDOW + jnp.arange(WINDOW + Q_BLOCK)
        mask_w = (kpos_w[None, :] <= t[:, None]) & (kpos_w[None, :] > t[:, None] - WINDOW) & (kpos_w[None, :] >= 0)
        s_w = jnp.einsum('bqghd,bkgd->bghqk', qblk, kw, preferred_element_type=jnp.float32) * scale
        p_w = masked_softmax(s_w, mask_w)
        o_w = jnp.einsum('bghqk,bkgd->bqghd', p_w, vw.astype(jnp.float32))
        o = gblk[:, :, 0, ..., None] * o_c + gblk[:, :, 1, ..., None] * o_s + gblk[:, :, 2, ..., None] * o_w
        return o.reshape(B, Q_BLOCK, H * DK).astype(x.dtype)

    out = lax.map(block_fn, (jnp.arange(nqb), q, gates))
    out = jnp.moveaxis(out, 0, 1).reshape(B, T, H * DK)
    return out @ w_o


def moe_ffn(h, router_w, router_b, w_gu, b_gu, w_down, b_down):
    B, T, D = h.shape
    x = h.reshape(-1, D)
    n = x.shape[0]
    logits = (x @ router_w + router_b).astype(jnp.float32)
    top_v, top_i = lax.top_k(logits, TOP_K)
    gates = jax.nn.softmax(top_v, axis=-1)
    flat_e = top_i.reshape(-1)
    flat_tok = jnp.repeat(jnp.arange(n), TOP_K)
    order = jnp.argsort(flat_e, stable=True)
    se = flat_e[order]
    counts = jnp.bincount(flat_e, length=N_EXPERTS)
    padded = ((counts + ROW_BLOCK - 1) // ROW_BLOCK) * ROW_BLOCK
    pad_end = jnp.cumsum(padded)
    pad_start = pad_end - padded
    start = jnp.cumsum(counts) - counts
    dest = pad_start[se] + (jnp.arange(n * TOP_K) - start[se])
    n_rows = ((n * TOP_K + ROW_BLOCK - 1) // ROW_BLOCK) * ROW_BLOCK + N_EXPERTS * ROW_BLOCK
    n_blk = n_rows // ROW_BLOCK
    tok_sorted = flat_tok[order]
    xs = jnp.zeros((n_rows, D), x.dtype).at[dest].set(x[tok_sorted])
    blk_e = jnp.minimum(jnp.searchsorted(pad_end, jnp.arange(n_blk) * ROW_BLOCK, side='right'), N_EXPERTS - 1)

    def expert_block(args):
        xb, e = args
        gu = xb @ w_gu[e] + b_gu[e]
        gate = jnp.minimum(gu[:, :D_EXPERT], SWIGLU_LIMIT)
        up = jnp.clip(gu[:, D_EXPERT:], -SWIGLU_LIMIT, SWIGLU_LIMIT)
        act = (up + 1.0) * (gate * jax.nn.sigmoid(gate * SWIGLU_ALPHA))
        return act @ w_down[e] + b_down[e]

    ys = lax.map(expert_block, (xs.reshape(n_blk, ROW_BLOCK, D), blk_e)).reshape(n_rows, D)
    w_sorted = gates.reshape(-1)[order]
    out = jnp.zeros((n, D), ys.dtype).at[tok_sorted].add(ys[dest] * w_sorted[:, None].astype(ys.dtype))
    return out.reshape(B, T, D)


def setup_inputs(seed: int = 0) -> dict:
    key = jax.random.key(seed)
    ks = iter(jax.random.split(key, 40))

    def nrm(shape, scale):
        return jax.random.normal(next(ks), shape, jnp.float32) * scale

    D = D_MODEL
    NA, NB = N_A_LAYERS, N_B_LAYERS
    HD = NSA_HEADS * NSA_HEAD_DIM
    G, DK = NSA_KV_GROUPS, NSA_HEAD_DIM
    E, F = N_EXPERTS, D_EXPERT
    inp = {}
    inp['x'] = nrm((BATCH, SEQ, D), 1.0)
    inp['ln_g'] = 1.0 + nrm((DEPTH, 2, D), 0.02)
    inp['ln_b'] = nrm((DEPTH, 2, D), 0.02)
    inp['rw_mu'] = jax.random.uniform(next(ks), (NA, 6, D), jnp.float32)
    inp['rw_w_rkv'] = nrm((NA, 3, D, D), D ** -0.5)
    inp['rw_w0'] = -1.0 + nrm((NA, D), 0.5)
    inp['rw_w1'] = nrm((NA, D, RW_DECAY_RANK), D ** -0.5)
    inp['rw_w2'] = nrm((NA, RW_DECAY_RANK, D), 0.5 * RW_DECAY_RANK ** -0.5)
    inp['rw_a0'] = nrm((NA, D), 0.5)
    inp['rw_a1'] = nrm((NA, D, RW_A_RANK), D ** -0.5)
    inp['rw_a2'] = nrm((NA, RW_A_RANK, D), 0.5 * RW_A_RANK ** -0.5)
    inp['rw_g1'] = nrm((NA, D, RW_GATE_RANK), D ** -0.5)
    inp['rw_g2'] = nrm((NA, RW_GATE_RANK, D), RW_GATE_RANK ** -0.5)
    inp['rw_k_k'] = 0.85 + nrm((NA, D), 0.05)
    inp['rw_k_a'] = 1.0 + nrm((NA, D), 0.05)
    inp['rw_r_k'] = nrm((NA, RW_HEADS, RW_HEAD_DIM), 0.1)
    inp['rw_lnx_g'] = 1.0 + nrm((NA, D), 0.02)
    inp['rw_lnx_b'] = nrm((NA, D), 0.02)
    inp['rw_w_o'] = nrm((NA, D, D), BETA * D ** -0.5)
    inp['nsa_w_kv'] = nrm((D, 6 * G * DK), D ** -0.5)
    inp['nsa_cmp_pe'] = nrm((2, CMP_LEN, DK), 0.02)
    inp['nsa_cmp_w1'] = nrm((2, CMP_LEN * DK, CMP_HIDDEN), (CMP_LEN * DK) ** -0.5)
    inp['nsa_cmp_b1'] = nrm((2, CMP_HIDDEN), 0.02)
    inp['nsa_cmp_w2'] = nrm((2, CMP_HIDDEN, DK), CMP_HIDDEN ** -0.5)
    inp['nsa_cmp_b2'] = nrm((2, DK), 0.02)
    inp['nsa_w_in'] = nrm((NB, D, HD + 3 * NSA_HEADS), D ** -0.5)
    inp['nsa_b_gate'] = nrm((NB, 3 * NSA_HEADS), 0.02)
    inp['nsa_w_o'] = nrm((NB, HD, D), BETA * HD ** -0.5)
    inp['moe_router_w'] = nrm((DEPTH, D, E), D ** -0.5)
    inp['moe_router_b'] = nrm((DEPTH, E), 0.01)
    inp['moe_w_gu'] = nrm((DEPTH, E, D, 2 * F), D ** -0.5)
    inp['moe_b_gu'] = nrm((DEPTH, E, 2 * F), 0.02)
    inp['moe_w_down'] = nrm((DEPTH, E, F, D), BETA * F ** -0.5)
    inp['moe_b_down'] = nrm((DEPTH, E, D), 0.02)
    return inp


def reference(x, ln_g, ln_b, rw_mu, rw_w_rkv, rw_w0, rw_w1, rw_w2, rw_a0, rw_a1, rw_a2, rw_g1, rw_g2,
              rw_k_k, rw_k_a, rw_r_k, rw_lnx_g, rw_lnx_b, rw_w_o, nsa_w_kv, nsa_cmp_pe, nsa_cmp_w1,
              nsa_cmp_b1, nsa_cmp_w2, nsa_cmp_b2, nsa_w_in, nsa_b_gate, nsa_w_o, moe_router_w,
              moe_router_b, moe_w_gu, moe_b_gu, moe_w_down, moe_b_down):
    shared = None
    for layer in range(DEPTH):
        if layer < N_A_LAYERS:
            i = layer
            mix = rwkv7_time_mix(x, rw_mu[i], rw_w_rkv[i], rw_w0[i], rw_w1[i], rw_w2[i], rw_a0[i], rw_a1[i],
                                 rw_a2[i], rw_g1[i], rw_g2[i], rw_k_k[i], rw_k_a[i], rw_r_k[i],
                                 rw_lnx_g[i], rw_lnx_b[i], rw_w_o[i])
        else:
            if layer == N_A_LAYERS:
                shared = nsa_shared_kv(x, nsa_w_kv, nsa_cmp_pe, nsa_cmp_w1, nsa_cmp_b1, nsa_cmp_w2, nsa_cmp_b2)
            j = layer - N_A_LAYERS
            mix = nsa_attention(x, shared, nsa_w_in[j], nsa_b_gate[j], nsa_w_o[j])
        x = layer_norm(ALPHA * x + mix, ln_g[layer, 0], ln_b[layer, 0])
        ffn = moe_ffn(x, moe_router_w[layer], moe_router_b[layer], moe_w_gu[layer], moe_b_gu[layer],
                      moe_w_down[layer], moe_b_down[layer])
        x = layer_norm(ALPHA * x + ffn, ln_g[layer, 1], ln_b[layer, 1])
    return x
```

```python
import ml_dtypes
import numpy as np
import concourse.bass as bass
import concourse.mybir as mybir
from concourse.bass_utils import run_bass_kernel_spmd

F32 = mybir.dt.float32
BF16 = mybir.dt.bfloat16
I32 = mybir.dt.int32
AF = mybir.ActivationFunctionType
ALU = mybir.AluOpType
AX = mybir.AxisListType


class Buf:
    __slots__ = ("w", "r", "name")

    def __init__(self, name=""):
        self.w = None
        self.r = {}
        self.name = name


class K:
    NDMA = 40

    def __init__(self, nc):
        self.nc = nc
        self.eng = {"pe": nc.tensor, "dve": nc.vector, "act": nc.scalar, "pool": nc.gpsimd, "sp": nc.sync}
        self.sem = {k: nc.alloc_semaphore(name="e_" + k) for k in self.eng}
        self.cnt = {k: 0 for k in self.eng}
        self.seen = {k: {} for k in self.eng}
        self.dsem = [nc.alloc_semaphore(name="d%d" % i) for i in range(self.NDMA)]
        self.duse = [0] * self.NDMA
        self.dnext = 0
        self.nins = 0

    def _semh(self, key):
        return self.sem[key] if isinstance(key, str) else self.dsem[key]

    def _wait(self, e, deps):
        eng = self.eng[e]
        seen = self.seen[e]
        for key, val in deps.items():
            if e == "pe" and key == "pe":
                continue
            if seen.get(key, 0) < val:
                eng.wait_ge(self._semh(key), val)
                seen[key] = val

    @staticmethod
    def _deps(reads, writes):
        d = {}

        def add(tok):
            if tok is not None and d.get(tok[0], 0) < tok[1]:
                d[tok[0]] = tok[1]
        for b in reads:
            add(b.w)
        for b in writes:
            add(b.w)
            for kv in b.r.items():
                add(kv)
        return d

    @staticmethod
    def _mark(tok, reads, writes):
        for b in reads:
            if b.r.get(tok[0], 0) < tok[1]:
                b.r[tok[0]] = tok[1]
        for b in writes:
            b.w = tok
            b.r = {}

    def op(self, e, fn, reads=(), writes=()):
        self._wait(e, self._deps(reads, writes))
        ins = fn(self.eng[e])
        self.cnt[e] += 1
        ins.then_inc(self.sem[e], 1)
        self._mark((e, self.cnt[e]), reads, writes)
        self.nins += 1

    def mm(self, out_buf, mms, reads, extra_writes=()):
        writes = (out_buf,) + tuple(extra_writes)
        self._wait("pe", self._deps(reads, writes))
        n = len(mms)
        for i, (o, l, r) in enumerate(mms):
            ins = self.nc.tensor.matmul(o, l, r, start=(i == 0), stop=(i == n - 1))
        self.cnt["pe"] += 1
        ins.then_inc(self.sem["pe"], 1)
        self._mark(("pe", self.cnt["pe"]), reads, writes)
        self.nins += n

    def pe_raw(self, fn, reads, writes):
        self._wait("pe", self._deps(reads, writes))
        ins = fn()
        self.cnt["pe"] += 1
        ins.then_inc(self.sem["pe"], 1)
        self._mark(("pe", self.cnt["pe"]), reads, writes)

    def dma(self, q, out, in_, reads=(), writes=(), **kw):
        slot = self.dnext
        self.dnext = (self.dnext + 1) % self.NDMA
        deps = self._deps(reads, writes)
        if self.duse[slot]:
            deps[slot] = max(deps.get(slot, 0), 16 * self.duse[slot])
        self._wait(q, deps)
        self.duse[slot] += 1
        self.eng[q].dma_start(out=out, in_=in_, **kw).then_inc(self.dsem[slot], 16)
        self._mark((slot, 16 * self.duse[slot]), reads, writes)
        self.nins += 1

    def finish(self, bufs):
        self._wait("sp", self._deps((), bufs))


def _coll(self, kind, groups, ins, outs, reads=(), writes=()):
    slot = self.dnext
    self.dnext = (self.dnext + 1) % self.NDMA
    deps = self._deps(reads, writes)
    if self.duse[slot]:
        deps[slot] = max(deps.get(slot, 0), 16 * self.duse[slot])
    self._wait("pool", deps)
    self.duse[slot] += 1
    self.nc.gpsimd.collective_compute(kind, ALU.bypass, replica_groups=groups, ins=ins, outs=outs).then_inc(self.dsem[slot], 16)
    self._mark((slot, 16 * self.duse[slot]), reads, writes)


K.coll = _coll


LN_EPS = 1e-5
GN_EPS = 64e-5
ALPHA = 4.0 ** 0.25


class Tn:
    def __init__(self, h, name):
        self.h = h
        self.b = Buf(name)

    def __getitem__(self, key):
        return self.h.ap()[key]


def mk(nc):
    def sb(name, shape, dt=F32):
        return Tn(nc.alloc_sbuf_tensor("s_" + name, shape, dt), name)

    def ps(name, shape, dt=F32):
        return Tn(nc.alloc_psum_tensor("p_" + name, shape, dt), name)
    return sb, ps


def build_A(T):
    nc = bass.Bass("TRN2", target_bir_lowering=False)
    k = K(nc)
    sb, ps = mk(nc)
    NCH = T // 128
    din = lambda n, s, dt=F32: nc.dram_tensor(n, s, dt, kind="ExternalInput").ap()
    xT = din("xT", [2048, T + 1])
    mu_d = din("mu", [128, 96])
    wrkv_d = din("wrkv", [2048, 1536])
    w1_d = din("w1", [2048, 448])
    w2_d = din("w2", [96, 512]); a2_d = din("a2", [96, 512]); g2_d = din("g2", [256, 512])
    vec_d = din("vecs", [7, 512])
    cst_d = din("cst", [128, 768])
    yg_d = nc.dram_tensor("yg", [T, 512], BF16, kind="ExternalOutput").ap()
    Bout = Buf("yg_d")
    Bin = Buf("in")

    cst = sb("cst", [128, 768]); mu = sb("mus", [128, 96])
    k.dma("sp", cst[:, :], cst_d[:, :], writes=[cst.b])
    k.dma("sp", mu[:, :], mu_d[:, :], writes=[mu.b])
    ident = cst[:, 0:128]; tri = cst[:, 128:256]; mask2 = cst[:, 256:512]; SL = cst[:, 512:640]; ones = cst[:, 640:768]
    vbc = sb("vbc", [128, 7, 512])
    for i in range(7):
        k.dma("sp", vbc[:, i, :], vec_d[i:i + 1, :].partition_broadcast(128), writes=[vbc.b])
    stage = sb("stage", [128, 1, 1536])
    wrkv = sb("wrkvb", [128, 16, 1536], BF16)
    wv = wrkv_d.rearrange("(c p) n -> p c n", p=128)
    for c4 in range(16):
        k.dma("sp", stage[:, :, :], wv[:, c4:c4 + 1, :], writes=[stage.b])
        k.op("dve", lambda e: e.tensor_copy(out=wrkv[:, c4:c4 + 1, :], in_=stage[:, :, :]), reads=[stage.b], writes=[wrkv.b])
    w1 = sb("w1b", [128, 16, 448], BF16)
    w1v = w1_d.rearrange("(c p) n -> p c n", p=128)
    for c4 in range(16):
        k.dma("sp", stage[:, :, 0:448], w1v[:, c4:c4 + 1, :], writes=[stage.b])
        k.op("dve", lambda e: e.tensor_copy(out=w1[:, c4:c4 + 1, :], in_=stage[:, :, 0:448]), reads=[stage.b], writes=[w1.b])
    w2 = sb("w2b", [128, 4, 512], BF16)
    for i, (src, rows) in enumerate([(w2_d[:, :], 96), (a2_d[:, :], 96), (g2_d[0:128, :], 128), (g2_d[128:256, :], 128)]):
        k.dma("sp", stage[0:rows, 0, 0:512], src, writes=[stage.b])
        k.op("dve", lambda e: e.tensor_copy(out=w2[0:rows, i, :], in_=stage[0:rows, 0, 0:512]), reads=[stage.b], writes=[w2.b])

    xt = [sb("xt%d" % i, [128, 16, 129]) for i in range(2)]
    dx = sb("dx", [128, 16, 128]); tmpx = sb("tmpx", [128, 16, 128])
    xm = [sb("xm%d" % i, [128, 16, 128], BF16) for i in range(2)]
    hT = sb("hT", [128, 4, 128], BF16)
    names = "r k v g logw a kk kkn k2 b G eG eGi eGp eGe rt at bt kt bb kb t1 t2".split()
    tm = {n: sb("tm_" + n, [128, 512]) for n in names}
    for a_, b_ in (("sq", "t1"), ("zw", "t2"), ("sg", "t1"), ("Gp", "t2"), ("yc", "eG"), ("y", "eGi")):
        tm[a_] = tm[b_]
    yg = sb("ygt", [128, 512], BF16)
    st8 = {n: sb("st_" + n, [128, 8]) for n in "ss rn s1 s2 rs rk".split()}
    gC = sb("gC", [128, 4])
    FM = [sb("FM%d" % i, [128, 4, 128]) for i in range(4)]
    ST = [sb("ST%d" % i, [128, 64]) for i in range(4)]
    UT2 = [sb("UT2_%d" % i, [128, 128]) for i in range(4)]
    NH = 2
    MN1 = [sb("MN1_%d" % i, [128, 256]) for i in range(NH)]
    MN2 = [sb("MN2_%d" % i, [128, 256]) for i in range(NH)]
    PT0 = [sb("PT0_%d" % i, [128, 128]) for i in range(NH)]
    PP = [[sb("PP%d_%d" % (i, j), [128, 256]) for j in range(6)] for i in range(NH)]
    Z = [[sb("Z%d_%d" % (i, j), [128, 64]) for j in range(2)] for i in range(NH)]
    P = [ps("P%d" % i, [128, 512]) for i in range(8)]
    pi = [0]

    def nps():
        pi[0] = (pi[0] + 1) % 8
        return P[pi[0]]

    for s in ST:
        k.op("pool", lambda e: e.memset(s[:, :], 0.0), writes=[s.b])

    def v3(t):
        return t[:, :].rearrange("p (h j) -> p h j", h=8)

    def bc8(t):
        return t[:, :].unsqueeze(2).to_broadcast([128, 8, 64])

    def tt(e, o, a, b, op, **kw):
        k.op(e, lambda en: en.tensor_tensor(out=o[:, :], in0=a[:, :], in1=b[:, :], op=op), reads=[a.b, b.b], writes=[o.b])

    def load_x(n):
        k.dma("sp", xt[n % 2][:, :, :], xT.rearrange("(c p) t -> p c t", p=128)[:, :, n * 128:n * 128 + 129], writes=[xt[n % 2].b])

    load_x(0)
    for n in range(NCH):
        X = xt[n % 2]
        if n + 1 < NCH:
            load_x(n + 1)
        k.op("pool", lambda e: e.tensor_tensor(out=dx[:, :, :], in0=X[:, :, 0:128], in1=X[:, :, 1:129], op=ALU.subtract), reads=[X.b], writes=[dx.b])
        mixed = {}

        def mix(m, slot, eng):
            o = xm[slot]
            k.op(eng, lambda e: e.tensor_tensor(out=tmpx[:, :, :], in0=dx[:, :, :], in1=mu[:, m * 16:(m + 1) * 16].unsqueeze(2).to_broadcast([128, 16, 128]), op=ALU.mult),
                 reads=[dx.b, mu.b], writes=[tmpx.b])
            k.op(eng, lambda e: e.tensor_tensor(out=o[:, :, :], in0=tmpx[:, :, :], in1=X[:, :, 1:129], op=ALU.add), reads=[tmpx.b, X.b], writes=[o.b])
            return o

        def proj_tm(o, c0, dst):
            p = nps()
            k.mm(p.b, [(p[:, :], o[:, c, :], wrkv[:, c, c0:c0 + 512]) for c in range(16)], reads=[o.b, wrkv.b])
            k.op("act", lambda e: e.copy(out=dst[:, :], in_=p[:, :]), reads=[p.b], writes=[dst.b])

        def proj_fm(o, c0, ncol, slot, func):
            p = nps()
            k.mm(p.b, [(p[0:ncol, 0:128], w1[:, c, c0:c0 + ncol], o[:, c, :]) for c in range(16)], reads=[o.b, w1.b])
            k.op("act", lambda e: e.activation(out=hT[0:ncol, slot, :], in_=p[0:ncol, 0:128], func=func), reads=[p.b], writes=[hT.b])

        o = mix(0, 0, "dve"); proj_tm(o, 0, tm["r"])
        o = mix(2, 1, "pool"); proj_tm(o, 512, tm["k"])
        o = mix(3, 0, "dve"); proj_tm(o, 1024, tm["v"])
        o = mix(1, 1, "pool"); proj_fm(o, 0, 96, 0, AF.Tanh)
        o = mix(4, 0, "dve"); proj_fm(o, 96, 96, 1, AF.Copy)
        o = mix(5, 1, "pool"); proj_fm(o, 192, 128, 2, AF.Sigmoid); proj_fm(o, 320, 128, 3, AF.Sigmoid)
        p = nps()
        k.mm(p.b, [(p[:, :], hT[0:96, 0, :], w2[0:96, 0, :])], reads=[hT.b, w2.b])
        k.op("dve", lambda e: e.tensor_tensor(out=tm["zw"][:, :], in0=p[:, :], in1=vbc[:, 0, :], op=ALU.add), reads=[p.b, vbc.b], writes=[tm["zw"].b])
        k.op("act", lambda e: e.activation(out=tm["sg"][:, :], in_=tm["zw"][:, :], func=AF.Sigmoid), reads=[tm["zw"].b], writes=[tm["sg"].b])
        k.op("pool", lambda e: e.tensor_scalar(out=tm["logw"][:, :], in0=tm["sg"][:, :], scalar1=-0.6065306597126334, scalar2=None, op0=ALU.mult),
             reads=[tm["sg"].b], writes=[tm["logw"].b])
        p = nps()
        k.mm(p.b, [(p[:, :], hT[0:96, 1, :], w2[0:96, 1, :])], reads=[hT.b, w2.b])
        k.op("dve", lambda e: e.tensor_tensor(out=tm["zw"][:, :], in0=p[:, :], in1=vbc[:, 1, :], op=ALU.add), reads=[p.b, vbc.b], writes=[tm["zw"].b])
        k.op("act", lambda e: e.activation(out=tm["a"][:, :], in_=tm["zw"][:, :], func=AF.Sigmoid), reads=[tm["zw"].b], writes=[tm["a"].b])
        p = nps()
        k.mm(p.b, [(p[:, :], hT[:, 2, :], w2[:, 2, :]), (p[:, :], hT[:, 3, :], w2[:, 3, :])], reads=[hT.b, w2.b])
        k.op("act", lambda e: e.copy(out=tm["g"][:, :], in_=p[:, :]), reads=[p.b], writes=[tm["g"].b])
        k.op("pool", lambda e: e.tensor_tensor(out=tm["kk"][:, :], in0=tm["k"][:, :], in1=vbc[:, 2, :], op=ALU.mult), reads=[tm["k"].b, vbc.b], writes=[tm["kk"].b])
        tt("pool", tm["sq"], tm["kk"], tm["kk"], ALU.mult)
        k.op("dve", lambda e: e.tensor_reduce(out=st8["ss"][:, :], in_=v3(tm["sq"]), axis=AX.X, op=ALU.add), reads=[tm["sq"].b], writes=[st8["ss"].b])
        k.op("act", lambda e: e.activation(out=st8["ss"][:, :], in_=st8["ss"][:, :], func=AF.Sqrt, bias=1e-24, scale=1.0), reads=[st8["ss"].b], writes=[st8["ss"].b])
        k.op("dve", lambda e: e.reciprocal(out=st8["rn"][:, :], in_=st8["ss"][:, :]), reads=[st8["ss"].b], writes=[st8["rn"].b])
        k.op("pool", lambda e: e.tensor_tensor(out=v3(tm["kkn"]), in0=v3(tm["kk"]), in1=bc8(st8["rn"]), op=ALU.mult), reads=[tm["kk"].b, st8["rn"].b], writes=[tm["kkn"].b])
        k.op("dve", lambda e: e.scalar_tensor_tensor(out=tm["t1"][:, :], in0=tm["a"][:, :], scalar=-1.0, in1=vbc[:, 3, :], op0=ALU.add, op1=ALU.mult),
             reads=[tm["a"].b, vbc.b], writes=[tm["t1"].b])
        k.op("dve", lambda e: e.scalar_tensor_tensor(out=tm["k2"][:, :], in0=tm["t1"][:, :], scalar=1.0, in1=tm["k"][:, :], op0=ALU.add, op1=ALU.mult),
             reads=[tm["t1"].b, tm["k"].b], writes=[tm["k2"].b])
        tt("pool", tm["b"], tm["kkn"], tm["a"], ALU.mult)
        pG = nps()
        k.mm(pG.b, [(pG[:, :], tri, tm["logw"][:, :])], reads=[cst.b, tm["logw"].b])
        pGC = nps()
        k.mm(pGC.b, [(pGC[:, :], ones, tm["logw"][:, :])], reads=[cst.b, tm["logw"].b])
        pg4 = nps()
        for hp in range(4):
            k.mm(pg4.b, [(pg4[:, hp:hp + 1], tm["logw"][:, hp * 128:(hp + 1) * 128], ones[:, 0:1])], reads=[cst.b, tm["logw"].b])
        k.op("act", lambda e: e.activation(out=gC[:, :], in_=pg4[:, 0:4], func=AF.Exp), reads=[pg4.b], writes=[gC.b])
        k.op("act", lambda e: e.copy(out=tm["G"][:, :], in_=pG[:, :]), reads=[pG.b], writes=[tm["G"].b])
        k.op("act", lambda e: e.activation(out=tm["eG"][:, :], in_=tm["G"][:, :], func=AF.Exp), reads=[tm["G"].b], writes=[tm["eG"].b])
        k.op("act", lambda e: e.activation(out=tm["eGi"][:, :], in_=tm["G"][:, :], func=AF.Exp, scale=-1.0), reads=[tm["G"].b], writes=[tm["eGi"].b])
        tt("dve", tm["Gp"], tm["G"], tm["logw"], ALU.subtract)
        k.op("act", lambda e: e.activation(out=tm["eGp"][:, :], in_=tm["Gp"][:, :], func=AF.Exp), reads=[tm["Gp"].b], writes=[tm["eGp"].b])
        k.op("dve", lambda e: e.tensor_tensor(out=tm["Gp"][:, :], in0=pGC[:, :], in1=tm["G"][:, :], op=ALU.subtract), reads=[pGC.b, tm["G"].b], writes=[tm["Gp"].b])
        k.op("act", lambda e: e.activation(out=tm["eGe"][:, :], in_=tm["Gp"][:, :], func=AF.Exp), reads=[tm["Gp"].b], writes=[tm["eGe"].b])
        tt("dve", tm["rt"], tm["r"], tm["eG"], ALU.mult)
        k.op("dve", lambda e: e.scalar_tensor_tensor(out=tm["at"][:, :], in0=tm["kkn"][:, :], scalar=-1.0, in1=tm["eGp"][:, :], op0=ALU.mult, op1=ALU.mult),
             reads=[tm["kkn"].b, tm["eGp"].b], writes=[tm["at"].b])
        tt("dve", tm["bt"], tm["b"], tm["eGi"], ALU.mult)
        tt("pool", tm["kt"], tm["k2"], tm["eGi"], ALU.mult)
        tt("dve", tm["bb"], tm["b"], tm["eGe"], ALU.mult)
        tt("pool", tm["kb"], tm["k2"], tm["eGe"], ALU.mult)
        for hp in range(4):
            p = nps()
            srcs = [tm["at"], tm["rt"], tm["bt"], tm["kt"]]

            def tr4():
                for xi, s in enumerate(srcs):
                    ins = nc.tensor.transpose(p[:, xi * 128:(xi + 1) * 128], s[:, hp * 128:(hp + 1) * 128], ident)
                return ins
            k.pe_raw(tr4, reads=[s.b for s in srcs] + [cst.b], writes=[p.b])
            k.op("act", lambda e: e.copy(out=FM[hp][:, :, :].rearrange("p a t -> p (a t)"), in_=p[:, :]), reads=[p.b], writes=[FM[hp].b])
        for h in range(8):
            hp, o_, hi = h // 2, 64 * (h % 2), h % NH
            sl = slice(o_, o_ + 64)
            cols = slice(h * 64, (h + 1) * 64)
            F = FM[hp]
            ar = F[sl, 0:2, :].rearrange("p a t -> p (a t)")
            p1 = nps(); k.mm(p1.b, [(p1[:, 0:256], F[sl, 2, :], ar)], reads=[F.b])
            k.op("dve", lambda e: e.tensor_tensor(out=MN1[hi][:, :], in0=p1[:, 0:256], in1=mask2, op=ALU.mult), reads=[p1.b, cst.b], writes=[MN1[hi].b])
            p2 = nps(); k.mm(p2.b, [(p2[:, 0:256], F[sl, 3, :], ar)], reads=[F.b])
            k.op("dve", lambda e: e.tensor_tensor(out=MN2[hi][:, :], in0=p2[:, 0:256], in1=mask2, op=ALU.mult), reads=[p2.b, cst.b], writes=[MN2[hi].b])
            p3 = nps(); k.mm(p3.b, [(p3[:, 0:128], F[sl, 0, :], F[sl, 2, :])], reads=[F.b])
            k.op("dve", lambda e: e.tensor_tensor(out=PT0[hi][:, :], in0=p3[:, 0:128], in1=SL, op=ALU.mult), reads=[p3.b, cst.b], writes=[PT0[hi].b])
            Pm = [(MN1[hi][:, 0:128], PT0[hi][:, :], [MN1[hi].b, PT0[hi].b])]
            for j in range(6):
                Pj, PTj, bj = Pm[j]
                p = nps()
                k.mm(p.b, [(p[:, 0:128], PTj, Pj)], reads=bj)
                k.mm(p.b, [(p[:, 128:256], Pj, PTj)], reads=bj)
                q = PP[hi][j]
                k.op("act", lambda e: e.copy(out=q[:, :], in_=p[:, 0:256]), reads=[p.b], writes=[q.b])
                Pm.append((q[:, 0:128], q[:, 128:256], [q.b]))
            p = nps()
            k.mm(p.b, [(p[:, 0:64], F[sl, 0, :], ST[hp][sl, :]), (p[:, 0:64], MN2[hi][:, 0:128], tm["v"][:, cols])], reads=[F.b, ST[hp].b, MN2[hi].b, tm["v"].b])
            zc = Z[hi][0]
            k.op("act", lambda e: e.copy(out=zc[:, :], in_=p[:, 0:64]), reads=[p.b], writes=[zc.b])
            for j in range(7):
                Pj, _, bj = Pm[j]
                p = nps()
                k.mm(p.b, [(p[:, 0:64], Pj, zc[:, :])], reads=bj + [zc.b])
                if j < 6:
                    zn = Z[hi][(j + 1) % 2]
                    k.op("dve", lambda e: e.tensor_tensor(out=zn[:, :], in0=p[:, 0:64], in1=zc[:, :], op=ALU.add), reads=[p.b, zc.b], writes=[zn.b])
                    zc = zn
                else:
                    k.op("dve", lambda e: e.tensor_tensor(out=UT2[hp][:, sl], in0=p[:, 0:64], in1=zc[:, :], op=ALU.add), reads=[p.b, zc.b], writes=[UT2[hp].b])
            p = nps()
            k.mm(p.b, [(p[:, 0:64], F[sl, 1, :], ST[hp][sl, :]), (p[:, 0:64], MN1[hi][:, 128:256], UT2[hp][:, sl]), (p[:, 0:64], MN2[hi][:, 128:256], tm["v"][:, cols])],
                 reads=[F.b, ST[hp].b, MN1[hi].b, UT2[hp].b, MN2[hi].b, tm["v"].b])
            k.op("act", lambda e: e.copy(out=tm["y"][:, cols], in_=p[:, 0:64]), reads=[p.b], writes=[tm["y"].b])
            if h % 2 == 1:
                hc = slice(hp * 128, (hp + 1) * 128)
                p = nps()
                k.mm(p.b, [(p[:, 0:128], tm["bb"][:, hc], UT2[hp][:, :]), (p[:, 0:128], tm["kb"][:, hc], tm["v"][:, hc])], reads=[tm["bb"].b, tm["kb"].b, UT2[hp].b, tm["v"].b])
                for half in range(2):
                    s2 = slice(64 * half, 64 * half + 64)
                    k.op("dve", lambda e: e.scalar_tensor_tensor(out=ST[hp][s2, :], in0=ST[hp][s2, :], scalar=gC[s2, hp:hp + 1], in1=p[s2, s2], op0=ALU.mult, op1=ALU.add),
                         reads=[ST[hp].b, gC.b, p.b], writes=[ST[hp].b])
        y = tm["y"]
        k.op("dve", lambda e: e.tensor_reduce(out=st8["s1"][:, :], in_=v3(y), axis=AX.X, op=ALU.add), reads=[y.b], writes=[st8["s1"].b])
        k.op("dve", lambda e: e.tensor_scalar(out=st8["s1"][:, :], in0=st8["s1"][:, :], scalar1=-1.0 / 64, scalar2=None, op0=ALU.mult), reads=[st8["s1"].b], writes=[st8["s1"].b])
        k.op("pool", lambda e: e.tensor_tensor(out=v3(tm["yc"]), in0=v3(y), in1=bc8(st8["s1"]), op=ALU.add), reads=[y.b, st8["s1"].b], writes=[tm["yc"].b])
        tt("pool", tm["t1"], tm["yc"], tm["yc"], ALU.mult)
        k.op("dve", lambda e: e.tensor_reduce(out=st8["s2"][:, :], in_=v3(tm["t1"]), axis=AX.X, op=ALU.add), reads=[tm["t1"].b], writes=[st8["s2"].b])
        k.op("act", lambda e: e.activation(out=st8["s2"][:, :], in_=st8["s2"][:, :], func=AF.Sqrt, bias=GN_EPS, scale=1.0 / 64), reads=[st8["s2"].b], writes=[st8["s2"].b])
        k.op("dve", lambda e: e.reciprocal(out=st8["rs"][:, :], in_=st8["s2"][:, :]), reads=[st8["s2"].b], writes=[st8["rs"].b])
        k.op("pool", lambda e: e.tensor_tensor(out=v3(tm["yc"]), in0=v3(tm["yc"]), in1=bc8(st8["rs"]), op=ALU.mult), reads=[tm["yc"].b, st8["rs"].b], writes=[tm["yc"].b])
        k.op("pool", lambda e: e.tensor_tensor(out=tm["yc"][:, :], in0=tm["yc"][:, :], in1=vbc[:, 5, :], op=ALU.mult), reads=[tm["yc"].b, vbc.b], writes=[tm["yc"].b])
        k.op("pool", lambda e: e.tensor_tensor(out=tm["yc"][:, :], in0=tm["yc"][:, :], in1=vbc[:, 6, :], op=ALU.add), reads=[tm["yc"].b, vbc.b], writes=[tm["yc"].b])
        tt("pool", tm["t2"], tm["r"], tm["k2"], ALU.mult)
        k.op("pool", lambda e: e.tensor_tensor(out=tm["t2"][:, :], in0=tm["t2"][:, :], in1=vbc[:, 4, :], op=ALU.mult), reads=[tm["t2"].b, vbc.b], writes=[tm["t2"].b])
        k.op("dve", lambda e: e.tensor_reduce(out=st8["rk"][:, :], in_=v3(tm["t2"]), axis=AX.X, op=ALU.add), reads=[tm["t2"].b], writes=[st8["rk"].b])
        k.op("pool", lambda e: e.tensor_tensor(out=v3(tm["t2"]), in0=v3(tm["v"]), in1=bc8(st8["rk"]), op=ALU.mult), reads=[tm["v"].b, st8["rk"].b], writes=[tm["t2"].b])
        tt("pool", tm["yc"], tm["yc"], tm["t2"], ALU.add)
        k.op("pool", lambda e: e.tensor_tensor(out=yg[:, :], in0=tm["yc"][:, :], in1=tm["g"][:, :], op=ALU.mult), reads=[tm["yc"].b, tm["g"].b], writes=[yg.b])
        k.dma("sp", yg_d[n * 128:(n + 1) * 128, :], yg[:, :], reads=[yg.b], writes=[Bout])
    k.finish([Bout])
    return nc


def consts_A():
    c = np.zeros((128, 768), np.float32)
    i = np.arange(128)
    c[:, 0:128] = np.eye(128)
    c[:, 128:256] = (i[:, None] <= i[None, :])
    c[:, 256:384] = (i[:, None] < i[None, :])
    c[:, 384:512] = (i[:, None] <= i[None, :])
    c[:, 512:640] = (i[:, None] > i[None, :])
    c[:, 640:768] = 1.0
    return c


def inputs_A(inp, T, core):
    b, j = core // 4, core % 4
    cs = slice(j * 512, (j + 1) * 512)
    x = inp["x"][b, :T]
    xT = np.zeros((2048, T + 1), np.float32)
    xT[:, 1:] = x.T
    mu = np.ascontiguousarray(inp["rw_mu"][0].reshape(6, 16, 128).transpose(2, 0, 1).reshape(128, 96))
    wrkv = np.concatenate([inp["rw_w_rkv"][0, i][:, cs] for i in range(3)], axis=1)
    w1 = np.concatenate([inp["rw_w1"][0], inp["rw_a1"][0], inp["rw_g1"][0]], axis=1)
    vecs = np.stack([inp["rw_w0"][0][cs], inp["rw_a0"][0][cs], inp["rw_k_k"][0][cs], inp["rw_k_a"][0][cs],
                     inp["rw_r_k"][0].reshape(-1)[cs], inp["rw_lnx_g"][0][cs], inp["rw_lnx_b"][0][cs]])
    return {"xT": xT, "mu": mu, "wrkv": np.ascontiguousarray(wrkv), "w1": np.ascontiguousarray(w1),
            "w2": np.ascontiguousarray(inp["rw_w2"][0][:, cs]), "a2": np.ascontiguousarray(inp["rw_a2"][0][:, cs]),
            "g2": np.ascontiguousarray(inp["rw_g2"][0][:, cs]), "vecs": np.ascontiguousarray(vecs), "cst": consts_A()}


DBG = 0


class Ctx:
    pass


def setup_stream(nc, k, sb):
    c = Ctx()
    c.stage = [sb("wst%d" % i, [128, 16, 256]) for i in range(2)]
    c.wb = [sb("wb%d" % i, [128, 16, 256], BF16) for i in range(3)]
    c.i = 0
    c.cast_eng = ["act", "pool", "dve"]
    return c


def wunit(k, c, src, ncols, src_buf=None, nch=16):
    i = c.i
    c.i += 1
    st = c.stage[i % 2]
    wb = c.wb[i % 3]
    k.dma("sp", st[:, 0:nch, 0:ncols], src.rearrange("(c p) n -> p c n", p=128), reads=[src_buf] if src_buf else [], writes=[st.b])
    eng = c.cast_eng[i % 3]
    if eng == "act":
        k.op("act", lambda e: e.copy(out=wb[:, 0:nch, 0:ncols], in_=st[:, 0:nch, 0:ncols]), reads=[st.b], writes=[wb.b])
    else:
        k.op(eng, lambda e: e.tensor_copy(out=wb[:, 0:nch, 0:ncols], in_=st[:, 0:nch, 0:ncols]), reads=[st.b], writes=[wb.b])
    return wb


def emit_post_moe(nc, k, sb, ps, P, nps, TL, E, d, tile_cb, blk_cb=None, mode="all", first=True):
    NB = TL // 512
    cstr = setup_stream(nc, k, sb)
    cst = sb("cstB", [128, 768])
    k.dma("sp", cst[:, :], d["cst"][:, :], writes=[cst.b])
    ident = cst[:, 0:128]
    lnbc = sb("lnbc", [128, 2, 2048])

    def load_ln(i0):
        for i in range(2):
            k.dma("sp", lnbc[:, i, :], d["lnv"][i0 + i:i0 + i + 1, :].partition_broadcast(128), writes=[lnbc.b])
    rw = sb("rw", [128, 16, 32])
    rbc = sb("rbc", [128, 32])
    if mode in ("all", "p1"):
        load_ln(0)
        k.dma("sp", rw[:, :, :], d["rw"].rearrange("(c p) n -> p c n", p=128), writes=[rw.b])
        k.dma("sp", rbc[:, :], d["rb"][0:1, :].partition_broadcast(128), writes=[rbc.b])
    gates = sb("gates", [128, TL // 128, 32])
    xTb = sb("xTb", [128, 16, 512], BF16)
    acc = sb("acc", [128, 4, 2048])
    xr = sb("xr", [128, 2048])
    xn = sb("xn", [128, 2048])
    xnT = sb("xnT", [128, 16, 128])
    x2Tb = sb("x2Tb", [128, 16, 512], BF16)
    actT = sb("actT", [128, 16, 512], BF16)
    e1 = sb("e1", [128, 512]); e2 = sb("e2", [128, 512]); e3 = sb("e3", [128, 512])
    bdbc = sb("bdbc", [128, 2048])
    bgu = sb("bgu", [128, 32])
    st1 = {n: sb("stB_" + n, [128, 1]) for n in "s1 s2 mean var rstd nmax ssum rs".split()}
    m8 = sb("m8", [128, 8]); lg = sb("lg", [128, 32]); ex = sb("ex", [128, 32]); msk = sb("msk", [128, 32])
    Bx1 = Buf("x1d"); Bx1T = Buf("x1Td"); Bin = Buf("inB")

    def layer_norm(src_ap, src_bufs, gi, dst):
        k.op("dve", lambda e: e.tensor_reduce(out=st1["s1"][:, :], in_=src_ap, axis=AX.X, op=ALU.add), reads=src_bufs, writes=[st1["s1"].b])
        k.op("pool", lambda e: e.tensor_tensor(out=dst[:, :], in0=src_ap, in1=src_ap, op=ALU.mult), reads=src_bufs, writes=[dst.b])
        k.op("dve", lambda e: e.tensor_reduce(out=st1["s2"][:, :], in_=dst[:, :], axis=AX.X, op=ALU.add), reads=[dst.b], writes=[st1["s2"].b])
        k.op("dve", lambda e: e.tensor_scalar(out=st1["mean"][:, :], in0=st1["s1"][:, :], scalar1=1.0 / 2048, scalar2=None, op0=ALU.mult), reads=[st1["s1"].b], writes=[st1["mean"].b])
        k.op("dve", lambda e: e.tensor_tensor(out=st1["var"][:, :], in0=st1["mean"][:, :], in1=st1["mean"][:, :], op=ALU.mult), reads=[st1["mean"].b], writes=[st1["var"].b])
        k.op("dve", lambda e: e.scalar_tensor_tensor(out=st1["var"][:, :], in0=st1["s2"][:, :], scalar=1.0 / 2048, in1=st1["var"][:, :], op0=ALU.mult, op1=ALU.subtract),
             reads=[st1["s2"].b, st1["var"].b], writes=[st1["var"].b])
        k.op("act", lambda e: e.activation(out=st1["var"][:, :], in_=st1["var"][:, :], func=AF.Sqrt, bias=LN_EPS, scale=1.0), reads=[st1["var"].b], writes=[st1["var"].b])
        k.op("dve", lambda e: e.reciprocal(out=st1["rstd"][:, :], in_=st1["var"][:, :]), reads=[st1["var"].b], writes=[st1["rstd"].b])
        k.op("dve", lambda e: e.tensor_scalar(out=dst[:, :], in0=src_ap, scalar1=st1["mean"][:, 0:1], scalar2=st1["rstd"][:, 0:1], op0=ALU.subtract, op1=ALU.mult),
             reads=src_bufs + [st1["mean"].b, st1["rstd"].b], writes=[dst.b])
        k.op("pool", lambda e: e.tensor_tensor(out=dst[:, :], in0=dst[:, :], in1=lnbc[:, gi, :], op=ALU.mult), reads=[dst.b, lnbc.b], writes=[dst.b])
        k.op("pool", lambda e: e.tensor_tensor(out=dst[:, :], in0=dst[:, :], in1=lnbc[:, gi + 1, :], op=ALU.add), reads=[dst.b, lnbc.b], writes=[dst.b])

    def transpose_rows(src, dst_f32, dst_bf, col0):
        for q4 in range(4):
            p = nps()

            def tr():
                for i in range(4):
                    c = q4 * 4 + i
                    ins = nc.tensor.transpose(p[:, i * 128:(i + 1) * 128], src[:, c * 128:(c + 1) * 128], ident)
                return ins
            k.pe_raw(tr, reads=[src.b, cst.b], writes=[p.b])
            pv = p[:, :].rearrange("p (a t) -> p a t", a=4)
            if dst_f32 is not None:
                k.op("act", lambda e: e.copy(out=dst_f32[:, q4 * 4:(q4 + 1) * 4, :], in_=pv), reads=[p.b], writes=[dst_f32.b])
                k.op("dve", lambda e: e.tensor_copy(out=dst_bf[:, q4 * 4:(q4 + 1) * 4, col0:col0 + 128], in_=dst_f32[:, q4 * 4:(q4 + 1) * 4, :]), reads=[dst_f32.b], writes=[dst_bf.b])
            else:
                k.op("dve", lambda e: e.tensor_copy(out=dst_bf[:, q4 * 4:(q4 + 1) * 4, col0:col0 + 128], in_=pv), reads=[p.b], writes=[dst_bf.b])

    for blk in (range(NB) if mode in ("all", "p1") else ()):
        t0 = blk * 512
        k.dma("sp", xTb[:, :, :], d["mixT"].rearrange("(c p) t -> p c t", p=128)[:, :, t0:t0 + 512], reads=[d["mixT_buf"]], writes=[xTb.b])
        for u in range(8):
            wb = wunit(k, cstr, d["wo"][:, u * 256:(u + 1) * 256], 256)
            for tk in range(4):
                p = nps()
                k.mm(p.b, [(p[:, 0:256], xTb[:, c, tk * 128:(tk + 1) * 128], wb[:, c, :]) for c in range(16)], reads=[xTb.b, wb.b])
                k.op("act", lambda e: e.copy(out=acc[:, tk, u * 256:(u + 1) * 256], in_=p[:, 0:256]), reads=[p.b], writes=[acc.b])
        for tk in range(4):
            ti = blk * 4 + tk
            r0 = t0 + tk * 128
            k.dma("sp", xr[:, :], d["xres"][r0:r0 + 128, :], writes=[xr.b])
            k.op("dve", lambda e: e.scalar_tensor_tensor(out=xr[:, :], in0=xr[:, :], scalar=ALPHA, in1=acc[:, tk, :], op0=ALU.mult, op1=ALU.add),
                 reads=[xr.b, acc.b], writes=[xr.b])
            layer_norm(xr[:, :], [xr.b], 0, xn)
            k.dma("sp", d["x1"][r0:r0 + 128, :], xn[:, :], reads=[xn.b], writes=[Bx1])
            if DBG == 1:
                tile_cb(ti, xn, r0)
                continue
            transpose_rows(xn, xnT, x2Tb, tk * 128)
            if DBG == 2:
                tile_cb(ti, xn, r0)
                continue
            p = nps()
            k.mm(p.b, [(p[:, 0:32], xnT[:, c, :], rw[:, c, :]) for c in range(16)], reads=[xnT.b, rw.b])
            k.op("dve", lambda e: e.tensor_tensor(out=lg[:, :], in0=p[:, 0:32], in1=rbc[:, :], op=ALU.add), reads=[p.b, rbc.b], writes=[lg.b])
            k.op("dve", lambda e: e.max(out=m8[:, :], in_=lg[:, :]), reads=[lg.b], writes=[m8.b])
            k.op("dve", lambda e: e.tensor_scalar(out=msk[:, :], in0=lg[:, :], scalar1=m8[:, 3:4], scalar2=None, op0=ALU.is_ge), reads=[lg.b, m8.b], writes=[msk.b])
            k.op("dve", lambda e: e.tensor_scalar(out=st1["nmax"][:, :], in0=m8[:, 0:1], scalar1=-1.0, scalar2=None, op0=ALU.mult), reads=[m8.b], writes=[st1["nmax"].b])
            k.op("act", lambda e: e.activation(out=ex[:, :], in_=lg[:, :], func=AF.Exp, bias=st1["nmax"][:, 0:1], scale=1.0), reads=[lg.b, st1["nmax"].b], writes=[ex.b])
            k.op("dve", lambda e: e.tensor_tensor(out=ex[:, :], in0=ex[:, :], in1=msk[:, :], op=ALU.mult), reads=[ex.b, msk.b], writes=[ex.b])
            k.op("dve", lambda e: e.tensor_reduce(out=st1["ssum"][:, :], in_=ex[:, :], axis=AX.X, op=ALU.add), reads=[ex.b], writes=[st1["ssum"].b])
            k.op("dve", lambda e: e.reciprocal(out=st1["rs"][:, :], in_=st1["ssum"][:, :]), reads=[st1["ssum"].b], writes=[st1["rs"].b])
            k.op("dve", lambda e: e.tensor_scalar(out=gates[:, ti, :], in0=ex[:, :], scalar1=st1["rs"][:, 0:1], scalar2=None, op0=ALU.mult), reads=[ex.b, st1["rs"].b], writes=[gates.b])
            if DBG == 3:
                tile_cb(ti, xn, r0)
        k.dma("sp", d["x1T"].rearrange("(c p) t -> p c t", p=128)[:, :, t0:t0 + 512], x2Tb[:, :, :], reads=[x2Tb.b], writes=[Bx1T])
        if mode == "p1":
            k.dma("sp", d["gates_o"][t0:t0 + 512, :].rearrange("(t p) e -> p t e", p=128), gates[:, blk * 4:(blk + 1) * 4, :], reads=[gates.b], writes=[Bx1T])

    if mode == "p1":
        k.finish([Bx1, Bx1T])
        return
    if mode == "all":
        load_ln(2)
    elif mode == "p3":
        load_ln(0)
    Bacc = Buf("accd")
    for blk in range(NB):
        t0 = blk * 512
        if mode in ("all", "mo"):
            k.dma("sp", xTb[:, :, :], d["x1T"].rearrange("(c p) t -> p c t", p=128)[:, :, t0:t0 + 512], reads=[Bx1T], writes=[xTb.b])
        if mode == "mo":
            k.dma("sp", gates[:, blk * 4:(blk + 1) * 4, 0:E], d["gts"][t0:t0 + 512, :].rearrange("(t p) e -> p t e", p=128), writes=[gates.b])
        if mode == "p3" or (mode == "mo" and not first):
            k.dma("sp", acc[:, :, :], d["accin"][t0:t0 + 512, :].rearrange("(t p) c -> p t c", p=128), writes=[acc.b])
        for e_ in (range(E) if mode in ("all", "mo") else ()):
            k.dma("sp", bgu[:, :], d["bgu"][e_], writes=[bgu.b])
            k.dma("sp", bdbc[:, :], d["bd"][e_:e_ + 1, :].partition_broadcast(128), writes=[bdbc.b])
            for fs in range(16):
                stg = cstr.stage[cstr.i % 2]; wb = cstr.wb[cstr.i % 3]; ci = cstr.i; cstr.i += 1
                wsrc = d["wgu"][e_].rearrange("(c p) n -> p c n", p=128)
                k.dma("sp", stg[:, :, 0:128], wsrc[:, :, fs * 128:(fs + 1) * 128], writes=[stg.b])
                k.dma("sp", stg[:, :, 128:256], wsrc[:, :, 2048 + fs * 128:2048 + (fs + 1) * 128], writes=[stg.b])
                ce = cstr.cast_eng[ci % 3]
                if ce == "act":
                    k.op("act", lambda e: e.copy(out=wb[:, :, :], in_=stg[:, :, :]), reads=[stg.b], writes=[wb.b])
                else:
                    k.op(ce, lambda e: e.tensor_copy(out=wb[:, :, :], in_=stg[:, :, :]), reads=[stg.b], writes=[wb.b])
                pg = nps()
                k.mm(pg.b, [(pg[:, :], wb[:, c, 0:128], xTb[:, c, :]) for c in range(16)], reads=[wb.b, xTb.b])
                pu = nps()
                k.mm(pu.b, [(pu[:, :], wb[:, c, 128:256], xTb[:, c, :]) for c in range(16)], reads=[wb.b, xTb.b])
                k.op("dve", lambda e: e.tensor_scalar(out=e1[:, :], in0=pg[:, :], scalar1=bgu[:, fs:fs + 1], scalar2=7.0, op0=ALU.add, op1=ALU.min), reads=[pg.b, bgu.b], writes=[e1.b])
                k.op("act", lambda e: e.activation(out=e2[:, :], in_=e1[:, :], func=AF.Sigmoid, scale=1.702), reads=[e1.b], writes=[e2.b])
                k.op("dve", lambda e: e.tensor_scalar(out=e3[:, :], in0=pu[:, :], scalar1=bgu[:, 16 + fs:17 + fs], scalar2=-7.0, op0=ALU.add, op1=ALU.max), reads=[pu.b, bgu.b], writes=[e3.b])
                k.op("pool", lambda e: e.tensor_scalar(out=e3[:, :], in0=e3[:, :], scalar1=7.0, scalar2=1.0, op0=ALU.min, op1=ALU.add), reads=[e3.b], writes=[e3.b])
                k.op("pool", lambda e: e.tensor_tensor(out=e1[:, :], in0=e1[:, :], in1=e2[:, :], op=ALU.mult), reads=[e1.b, e2.b], writes=[e1.b])
                k.op("pool", lambda e: e.tensor_tensor(out=actT[:, fs, :], in0=e1[:, :], in1=e3[:, :], op=ALU.mult), reads=[e1.b, e3.b], writes=[actT.b])
            for u in range(8):
                wb = wunit(k, cstr, d["wd"][e_][:, u * 256:(u + 1) * 256], 256)
                for tk in range(4):
                    ti = blk * 4 + tk
                    p = nps()
                    k.mm(p.b, [(p[:, 0:256], actT[:, c, tk * 128:(tk + 1) * 128], wb[:, c, :]) for c in range(16)], reads=[actT.b, wb.b])
                    k.op("dve", lambda e: e.tensor_tensor(out=e1[:, 0:256], in0=p[:, 0:256], in1=bdbc[:, u * 256:(u + 1) * 256], op=ALU.add), reads=[p.b, bdbc.b], writes=[e1.b])
                    av = acc[:, tk, u * 256:(u + 1) * 256]
                    if e_ == 0 and (mode == "all" or first):
                        k.op("dve", lambda e: e.tensor_scalar(out=av, in0=e1[:, 0:256], scalar1=gates[:, ti, e_:e_ + 1], scalar2=None, op0=ALU.mult), reads=[e1.b, gates.b], writes=[acc.b])
                    else:
                        k.op("dve", lambda e: e.scalar_tensor_tensor(out=av, in0=e1[:, 0:256], scalar=gates[:, ti, e_:e_ + 1], in1=av, op0=ALU.mult, op1=ALU.add),
                             reads=[e1.b, gates.b, acc.b], writes=[acc.b])
        if mode == "mo":
            k.dma("sp", d["accout"][t0:t0 + 512, :].rearrange("(t p) c -> p t c", p=128), acc[:, :, :], reads=[acc.b], writes=[Bacc])
            continue
        for tk in range(4):
            ti = blk * 4 + tk
            r0 = t0 + tk * 128
            k.dma("sp", xr[:, :], d["x1"][r0:r0 + 128, :], reads=[Bx1], writes=[xr.b])
            k.op("dve", lambda e: e.scalar_tensor_tensor(out=xr[:, :], in0=xr[:, :], scalar=ALPHA, in1=acc[:, tk, :], op0=ALU.mult, op1=ALU.add),
                 reads=[xr.b, acc.b], writes=[xr.b])
            layer_norm(xr[:, :], [xr.b], 0, xn)
            tile_cb(ti, xn, r0)
            if blk_cb is not None and DBG != 4:
                transpose_rows(xn, None, x2Tb, tk * 128)
        if blk_cb is not None and DBG != 4:
            blk_cb(blk, x2Tb, cstr)
    if mode == "mo":
        k.finish([Bacc])


QSCALE = 128 ** -0.5


def build_P3(TL, nsa):
    nc = bass.Bass("TRN2", target_bir_lowering=False)
    k = K(nc)
    sb, ps = mk(nc)
    P = [ps("P%d" % i, [128, 512]) for i in range(8)]
    pi = [0]

    def nps():
        pi[0] = (pi[0] + 1) % 8
        return P[pi[0]]
    din = lambda n, s, dt=F32: nc.dram_tensor(n, s, dt, kind="ExternalInput").ap()
    dout = lambda n, s, dt=F32: nc.dram_tensor(n, s, dt, kind="ExternalOutput").ap()
    d = {"lnv": din("lnv", [2, 2048]), "cst": din("cst", [128, 768]), "x1": din("x1", [TL, 2048]), "accin": din("accin", [TL, 2048])}
    x2_o = dout("x2", [TL, 2048])
    if nsa:
        wkv = din("wkv", [2048, 1536]); win = din("win", [2048, 2096]); bgate = din("bgate", [1, 48])
        kvfm_o = dout("kvfm", [1024, TL], BF16); kvtm_o = dout("kvtm", [TL, 512], BF16)
        qT_o = dout("qT", [2048, TL], BF16); gts_o = dout("gts", [TL, 48])
    Bo = Buf("outs")
    ofm = [sb("ofm%d" % i, [128, 512], BF16) for i in range(2)]
    otm = [sb("otm%d" % i, [128, 256], BF16) for i in range(2)]
    og = sb("og", [128, 48]); bgbc = sb("bgbc", [128, 48])
    if nsa:
        k.dma("sp", bgbc[:, :], bgate[0:1, :].partition_broadcast(128), writes=[bgbc.b])
    cnt = [0]

    def tile_cb(ti, xn, r0):
        k.dma("sp", x2_o[r0:r0 + 128, :], xn[:, :], reads=[xn.b], writes=[Bo])

    def blk_cb(blk, x2Tb, cstr):
        t0 = blk * 512

        def fm_unit(src, dst_rows0, scale):
            wb = wunit(k, cstr, src, 256)
            for half in range(2):
                p = nps()
                k.mm(p.b, [(p[:, :], wb[:, c, half * 128:(half + 1) * 128], x2Tb[:, c, :]) for c in range(16)], reads=[wb.b, x2Tb.b])
                o = ofm[cnt[0] % 2]; cnt[0] += 1
                k.op("act", lambda e: e.activation(out=o[:, :], in_=p[:, :], func=AF.Copy, scale=scale), reads=[p.b], writes=[o.b])
                k.dma("sp", dst_rows0[half * 128:(half + 1) * 128, t0:t0 + 512], o[:, :], reads=[o.b], writes=[Bo])
        for ui, col0 in enumerate([0, 256, 512, 1024]):
            fm_unit(wkv[:, col0:col0 + 256], kvfm_o[ui * 256:(ui + 1) * 256, :], 1.0)
        for u in range(8):
            fm_unit(win[:, u * 256:(u + 1) * 256], qT_o[u * 256:(u + 1) * 256, :], QSCALE)
        for vi, col0 in enumerate([768, 1280]):
            wb = wunit(k, cstr, wkv[:, col0:col0 + 256], 256)
            for tk in range(4):
                p = nps()
                k.mm(p.b, [(p[:, 0:256], x2Tb[:, c, tk * 128:(tk + 1) * 128], wb[:, c, :]) for c in range(16)], reads=[wb.b, x2Tb.b])
                o = otm[cnt[0] % 2]; cnt[0] += 1
                k.op("act", lambda e: e.copy(out=o[:, :], in_=p[:, 0:256]), reads=[p.b], writes=[o.b])
                k.dma("sp", kvtm_o[t0 + tk * 128:t0 + (tk + 1) * 128, vi * 256:(vi + 1) * 256], o[:, :], reads=[o.b], writes=[Bo])
        wb = wunit(k, cstr, win[:, 2048:2096], 48)
        for tk in range(4):
            p = nps()
            k.mm(p.b, [(p[:, 0:48], x2Tb[:, c, tk * 128:(tk + 1) * 128], wb[:, c, 0:48]) for c in range(16)], reads=[wb.b, x2Tb.b])
            k.op("dve", lambda e: e.tensor_tensor(out=og[:, :], in0=p[:, 0:48], in1=bgbc[:, :], op=ALU.add), reads=[p.b, bgbc.b], writes=[og.b])
            k.op("act", lambda e: e.activation(out=og[:, :], in_=og[:, :], func=AF.Sigmoid), reads=[og.b], writes=[og.b])
            k.dma("sp", gts_o[t0 + tk * 128:t0 + (tk + 1) * 128, :], og[:, :], reads=[og.b], writes=[Bo])

    emit_post_moe(nc, k, sb, ps, P, nps, TL, 0, d, tile_cb, blk_cb if nsa else None, mode="p3")
    k.finish([Bo])
    return nc


NEG = -30000.0


def build_P1(TL):
    nc = bass.Bass("TRN2", target_bir_lowering=False)
    k = K(nc)
    sb, ps = mk(nc)
    P = [ps("P%d" % i, [128, 512]) for i in range(8)]
    pi = [0]

    def nps():
        pi[0] = (pi[0] + 1) % 8
        return P[pi[0]]
    din = lambda n, s, dt=F32: nc.dram_tensor(n, s, dt, kind="ExternalInput").ap()
    dout = lambda n, s, dt=F32: nc.dram_tensor(n, s, dt, kind="ExternalOutput").ap()
    d = {"mixT": din("mixT", [2048, TL], BF16), "xres": din("xres", [TL, 2048]), "wo": din("wo", [2048, 2048]),
         "lnv": din("lnv", [2, 2048]), "rw": din("rw", [2048, 32]), "rb": din("rb", [1, 32]),
         "cst": din("cst", [128, 768]), "mixT_buf": Buf("mixT"),
         "x1": dout("x1", [TL, 2048]), "x1T": dout("x1T", [2048, TL], BF16), "gates_o": dout("gates", [TL, 32])}
    emit_post_moe(nc, k, sb, ps, P, nps, TL, 0, d, None, None, mode="p1")
    return nc


def build_MO(TL, EG, first):
    nc = bass.Bass("TRN2", target_bir_lowering=False)
    k = K(nc)
    sb, ps = mk(nc)
    P = [ps("P%d" % i, [128, 512]) for i in range(8)]
    pi = [0]

    def nps():
        pi[0] = (pi[0] + 1) % 8
        return P[pi[0]]
    din = lambda n, s, dt=F32: nc.dram_tensor(n, s, dt, kind="ExternalInput").ap()
    d = {"x1T": din("x1T", [2048, TL], BF16), "gts": din("gts", [TL, EG]), "cst": din("cst", [128, 768]),
         "wgu": din("wgu", [EG, 2048, 4096]), "bgu": din("bgu", [EG, 128, 32]), "wd": din("wd", [EG, 2048, 2048]), "bd": din("bd", [EG, 2048]),
         "accout": nc.dram_tensor("acc", [TL, 2048], F32, kind="ExternalOutput").ap()}
    if not first:
        d["accin"] = din("accin", [TL, 2048])
    emit_post_moe(nc, k, sb, ps, P, nps, TL, EG, d, None, None, mode="mo", first=first)
    return nc


def consts_C(T):
    NS = T // 64
    NC = T // 16 - 1
    NCT = (NC + 127) // 128
    bf = ml_dtypes.bfloat16
    i = np.arange(128)
    cb = np.zeros((128, 128 + 512 + 512 + 64 * 128), np.float32)
    cb[:, 0:128] = np.eye(128)
    causal = np.where(i[:, None] > i[None, :], NEG, 0.0)
    lower = np.where(i[:, None] <= i[None, :], NEG, 0.0)
    cb[:, 128:640] = np.tile(causal, (1, 4))
    cb[:, 640:1152] = np.tile(lower, (1, 4))
    for pat in range(64):
        e = np.zeros((128, 128), np.float32)
        key = np.arange(128)
        r = 2 * pat + key // 64
        ok = r < 128
        e[r[ok], key[ok]] = 1.0
        cb[:, 1152 + pat * 128:1152 + (pat + 1) * 128] = e
    q = np.arange(128)
    lim = np.floor((q - 31) / 16.0)
    zc = np.zeros((33, 352), np.float32)
    for k_ in range(33):
        zc[k_, 160 + k_] = 1.0
    ovl = np.zeros((NCT * 128, NS + 1), np.float32)
    c = np.arange(NC)
    s = np.arange(NS)
    c_lo = c * 16
    s_lo = s * 64
    ovl[:NC, :NS] = ((c_lo[:, None] < s_lo[None, :] + 64) & (c_lo[:, None] + 32 > s_lo[None, :]))
    ovl[:NC, NS] = 1.0
    pb = np.zeros((128, 2 * NS), np.float32)
    for x in range(2 * NS):
        sp = x - NS
        if sp <= -2:
            v = np.zeros(128)
        elif sp == -1:
            v = np.where(q < 64, 1e4, 0.0)
        elif sp == 0:
            v = np.full(128, 1e4)
        elif sp == 1:
            v = np.where(q >= 64, 1e4, -1e4)
        else:
            v = np.full(128, -1e4)
        pb[:, x] = v
    out = []
    allneg = np.full((128, 512), NEG, np.float32)
    zero = np.zeros((128, 512), np.float32)
    for j in range(4):
        dm = np.concatenate([zero if r < j else (cb[:, 128:640] if r == j else allneg) for r in range(4)], 1)
        wm = np.concatenate([allneg if r < j else (cb[:, 640:1152] if r == j else (zero if r < j + 4 else (cb[:, 128:640] if r == j + 4 else allneg)))
                             for r in range(8)], 1)
        kk = np.arange(33)[:, None] - 1 - 8 * j
        bc = np.tile(np.where(kk > lim[None, :], NEG, 0.0), (1, 4)).astype(np.float32)
        pbj = np.zeros_like(pb)
        pbj[:, 2 * j:] = pb[:, :2 * NS - 2 * j]
        out.append({"cb": cb.astype(bf), "zc": zc.astype(bf), "ovl": np.ascontiguousarray(ovl.reshape(NCT, 128, NS + 1).transpose(1, 0, 2)).astype(bf),
                    "pb": pbj, "dm": dm.astype(bf), "wm": wm.astype(bf), "bc": bc.astype(bf)})
    return out


def barrier(k):
    deps = {e: c for e, c in k.cnt.items() if c}
    for s_, u in enumerate(k.duse):
        if u:
            deps[s_] = 16 * u
    for e in k.eng:
        k._wait(e, dict(deps))


def build_C(T):
    TL = T // 4
    NQ = TL // 128
    NS = T // 64
    NC = T // 16 - 1
    NCT = (NC + 127) // 128
    NKT = T // 128
    W = 128 + NS + 1
    NSC = (NS + 127) // 128
    SW = min(NS, 128)
    nc = bass.Bass("TRN2", target_bir_lowering=False)
    k = K(nc)
    din = lambda n, s, dt=F32: nc.dram_tensor(n, s, dt, kind="ExternalInput").ap()
    kvfm = din("kvfm", [1024, T], BF16); vtm = din("vtm", [T, 512], BF16)
    qT = din("qT", [2048, TL], BF16); gts = din("gts", [TL, 48])
    pe_d = din("peT", [2, 128, 32]); w1_d = din("cw1", [2, 4096, 256]); b1_d = din("cb1", [2, 128, 2]); w2_d = din("cw2", [2, 256, 128])
    b2_d = din("cb2", [2, 128])
    cb_d = din("cb", [128, 1152 + 8192], BF16); zc_d = din("zc", [33, 352], BF16); ovl_d = din("ovl", [128, NCT, NS + 1], BF16)
    pb_d = din("pb", [128, 2 * NS]); dm_d = din("dm", [128, 2048], BF16); wm_d = din("wm", [128, 4096], BF16); bc_d = din("bc", [33, 512], BF16)
    cst_d = din("cst", [128, 768])
    dout = lambda n, s, dt=F32: nc.dram_tensor(n, s, dt, kind="ExternalOutput").ap()
    d = {"xres": din("xres", [TL, 2048]), "wo": din("wo", [2048, 2048]),
         "lnv": din("lnv", [2, 2048]), "rw": din("rw", [2048, 32]), "rb": din("rb", [1, 32]),
         "cst": cst_d, "mixT_buf": Buf("mixT"), "mixT": nc.dram_tensor("mixTs", [2048, TL], BF16).ap(),
         "x1": dout("x1", [TL, 2048]), "x1T": dout("x1T", [2048, TL], BF16), "gates_o": dout("gates", [TL, 32])}
    dbg_o = None

    Bo = Buf("outs")
    from contextlib import ExitStack
    with ExitStack() as es:
        def sb(name, shape, dt=F32):
            return Tn(es.enter_context(nc.sbuf_tensor("s_" + name, shape, dt)), name)

        def psf(name, shape, dt=F32):
            return Tn(es.enter_context(nc.psum_tensor("p_" + name, shape, dt)), name)
        P = [psf("P%d" % i, [128, 512]) for i in range(5)]
        ACC = [psf("ACC%d" % i, [128, 512]) for i in range(3)]
        pi = [0]

        def nps():
            pi[0] = (pi[0] + 1) % 5
            return P[pi[0]]
        cb = sb("cb", [128, 1152 + 8192], BF16); k.dma("sp", cb[:, :], cb_d[:, :], writes=[cb.b])
        zc = sb("zc", [33, 352], BF16); k.dma("sp", zc[:, :], zc_d[:, :], writes=[zc.b])
        bc = sb("bc", [33, 512], BF16); k.dma("sp", bc[:, :], bc_d[:, :], writes=[bc.b])
        pb = sb("pb", [128, 2 * NS]); k.dma("sp", pb[:, :], pb_d[:, :], writes=[pb.b])
        dm = sb("dm", [128, 2048], BF16); k.dma("sp", dm[:, :], dm_d[:, :], writes=[dm.b])
        wm = sb("wm", [128, 4096], BF16); k.dma("sp", wm[:, :], wm_d[:, :], writes=[wm.b])
        cf = sb("cf", [128, 128]); k.dma("sp", cf[:, :], cst_d[:, 0:128], writes=[cf.b])
        identb = cb[:, 0:128]
        KCg = sb("KC", [128, NCT * 128], BF16)
        VCOg = sb("VCO", [128, NCT, W], BF16)
        KSg = sb("KS", [128, T], BF16)
        VS = sb("VS", [128, NKT, 129], BF16)
        k.op("pool", lambda e: e.memset(VS[:, :, 128:129], 1.0), writes=[VS.b])
        w1b = sb("w1b", [128, 32, 256], BF16); w1s = sb("w1s", [128, 8, 256])
        w2b = sb("w2b", [128, 2, 128], BF16); w2s = sb("w2s", [128, 2, 128])
        peT = sb("peT", [128, 32], BF16); pes = sb("pes", [128, 32]); b1t = sb("b1t", [128, 2]); bb = sb("bb", [128, 2])
        b2c = sb("b2c", [128, 1]); b2r = sb("b2r", [128, 128])
        hdn = sb("hdn", [128, 2, 512], BF16)
        ksrc = sb("ksrc", [128, T], BF16)
        ET = sb("ET", [128, NCT, 1024], BF16)
        qg = sb("qg", [128, 8, 128], BF16)
        gt = sb("gt", [128, 48])
        O = sb("O", [128, 1024])
        imp = sb("imp", [128, NS]); sc2 = sb("sc2", [128, NS]); selb = sb("selb", [128, NS])
        m8a = sb("m8a", [128, 8]); m8b = sb("m8b", [128, 8])
        selT4 = sb("selT4", [128, NSC, 4, 128], BF16)
        PT = [sb("PT%d" % i, [128, 512], BF16) for i in range(3)]
        pti = [0]
        rd = sb("rd", [128, 1]); coef = sb("coef", [128, 1])
        kwt = sb("kwt", [128, 8, 128], BF16)
        vwt = sb("vwt", [128, 8, 129], BF16)
        oT = sb("oT", [128, 8, 128], BF16)
        k.op("pool", lambda e: e.memset(vwt[:, :, 128:129], 1.0), writes=[vwt.b])

        def npt():
            pti[0] = (pti[0] + 1) % 3
            return PT[pti[0]]

        def finish_head(acc_ap, acc_buf, br, g, hh, first):
            col = br * 16 + g * 8 + hh
            hc = slice(hh * 128, (hh + 1) * 128)
            k.op("dve", lambda e: e.tensor_scalar(out=rd[:, :], in0=acc_ap[:, 128:129] if br else acc_ap[:, W - 1:W], scalar1=1e-30, scalar2=None, op0=ALU.max), reads=[acc_buf], writes=[rd.b])
            k.op("dve", lambda e: e.reciprocal(out=rd[:, :], in_=rd[:, :]), reads=[rd.b], writes=[rd.b])
            if br == 0:
                if hh == 0:
                    k.op("dve", lambda e: e.tensor_scalar(out=imp[:, :], in0=acc_ap[:, 128:128 + NS], scalar1=rd[:, 0:1], scalar2=None, op0=ALU.mult), reads=[acc_buf, rd.b], writes=[imp.b])
                else:
                    k.op("dve", lambda e: e.scalar_tensor_tensor(out=imp[:, :], in0=acc_ap[:, 128:128 + NS], scalar=rd[:, 0:1], in1=imp[:, :], op0=ALU.mult, op1=ALU.add),
                         reads=[acc_buf, rd.b, imp.b], writes=[imp.b])
            k.op("dve", lambda e: e.tensor_tensor(out=coef[:, :], in0=rd[:, :], in1=gt[:, col:col + 1], op=ALU.mult), reads=[rd.b, gt.b], writes=[coef.b])
            if first:
                k.op("dve", lambda e: e.tensor_scalar(out=O[:, hc], in0=acc_ap[:, 0:128], scalar1=coef[:, 0:1], scalar2=None, op0=ALU.mult), reads=[acc_buf, coef.b], writes=[O.b])
            else:
                k.op("dve", lambda e: e.scalar_tensor_tensor(out=O[:, hc], in0=acc_ap[:, 0:128], scalar=coef[:, 0:1], in1=O[:, hc], op0=ALU.mult, op1=ALU.add),
                     reads=[acc_buf, coef.b, O.b], writes=[O.b])

        def dense_branch(br, g, tiles):
            nt = len(tiles)
            for ti_, (kap, kbufs, vap, vbufs, masks) in enumerate(tiles):
                pts = []
                for hf in range(2):
                    p = nps()
                    mms = [(p[:, 0:512], kap, qg[:, hf * 4:(hf + 1) * 4, :].rearrange("p h q -> p (h q)"))]
                    rb_ = list(kbufs) + [qg.b]
                    for (ml, mr, mb) in masks:
                        mms.append((p[:, 0:512], ml, mr))
                        rb_ += mb
                    k.mm(p.b, mms, reads=rb_)
                    pt = npt()
                    k.op("act", lambda e: e.activation(out=pt[:, :], in_=p[:, 0:512], func=AF.Exp), reads=[p.b], writes=[pt.b])
                    pts.append(pt)
                for hh in range(8):
                    a = ACC[hh // 3]
                    o0 = (hh % 3) * 160
                    pt = pts[hh // 4]
                    k._wait("pe", k._deps([pt.b] + list(vbufs), [a.b]))
                    ins = nc.tensor.matmul(a[:, o0:o0 + 129], pt[:, (hh % 4) * 128:(hh % 4 + 1) * 128], vap,
                                           start=(ti_ == 0 and hh % 3 == 0), stop=(ti_ == nt - 1 and (hh % 3 == 2 or hh == 7)))
                    k.cnt["pe"] += 1
                    ins.then_inc(k.sem["pe"], 1)
                    k._mark(("pe", k.cnt["pe"]), [pt.b] + list(vbufs), [a.b])
            for hh in range(8):
                a = ACC[hh // 3]
                o0 = (hh % 3) * 160
                finish_head(a[:, o0:o0 + 129], a.b, br, g, hh, False)

        for g in range(2):
            k.op("pool", lambda e: e.memset(KCg[:, :], 0.0), writes=[KCg.b])
            k.op("pool", lambda e: e.memset(VCOg[:, :, :], 0.0), writes=[VCOg.b])
            k.dma("sp", VCOg[:, :, 128:W], ovl_d[:, :, :], writes=[VCOg.b])
            for ii in range(2):
                for l4 in range(4):
                    k.dma("sp", w1s[:, :, :], w1_d[ii].rearrange("(l p) n -> p l n", p=128)[:, l4 * 8:(l4 + 1) * 8, :], writes=[w1s.b])
                    k.op("dve", lambda e: e.tensor_copy(out=w1b[:, l4 * 8:(l4 + 1) * 8, :], in_=w1s[:, :, :]), reads=[w1s.b], writes=[w1b.b])
                k.dma("sp", w2s[:, :, :], w2_d[ii].rearrange("(h p) n -> p h n", p=128), writes=[w2s.b])
                k.op("dve", lambda e: e.tensor_copy(out=w2b[:, :, :], in_=w2s[:, :, :]), reads=[w2s.b], writes=[w2b.b])
                k.dma("sp", pes[:, :], pe_d[ii], writes=[pes.b])
                k.op("dve", lambda e: e.tensor_copy(out=peT[:, :], in_=pes[:, :]), reads=[pes.b], writes=[peT.b])
                k.dma("sp", b1t[:, :], b1_d[ii], writes=[b1t.b])
                k.dma("sp", b2c[:, :], b2_d[ii:ii + 1, :].rearrange("o p -> p o"), writes=[b2c.b], allow_slow_non_contiguous=True)
                k.dma("sp", b2r[:, :], b2_d[ii:ii + 1, :].partition_broadcast(128), writes=[b2r.b])
                for half in range(2):
                    p = nps()
                    k.mm(p.b, [(p[:, 0:1], w1b[:, l, half * 128:(half + 1) * 128], peT[:, l:l + 1]) for l in range(32)], reads=[w1b.b, peT.b])
                    k.op("dve", lambda e: e.tensor_tensor(out=bb[:, half:half + 1], in0=p[:, 0:1], in1=b1t[:, half:half + 1], op=ALU.add), reads=[p.b, b1t.b], writes=[bb.b])
                k.dma("sp", ksrc[:, :], kvfm[(ii * 2 + g) * 128:(ii * 2 + g + 1) * 128, :], writes=[ksrc.b])
                for c0 in range(0, NC, 512):
                    n = min(512, NC - c0)
                    for half in range(2):
                        p = nps()
                        k.mm(p.b, [(p[:, 0:n], w1b[:, l, half * 128:(half + 1) * 128], ksrc[:, l + 16 * c0:l + 16 * (c0 + n - 1) + 1:16]) for l in range(32)], reads=[w1b.b, ksrc.b])
                        k.op("act", lambda e: e.activation(out=hdn[:, half, 0:n], in_=p[:, 0:n], func=AF.Gelu_apprx_tanh, bias=bb[:, half:half + 1], scale=1.0),
                             reads=[p.b, bb.b], writes=[hdn.b])
                    if ii == 0:
                        p = nps()
                        k.mm(p.b, [(p[:, 0:n], w2b[:, half, :], hdn[:, half, 0:n]) for half in range(2)], reads=[w2b.b, hdn.b])
                        k.op("act", lambda e: e.activation(out=KCg[:, c0:c0 + n], in_=p[:, 0:n], func=AF.Identity, bias=b2c[:, 0:1], scale=1.0), reads=[p.b, b2c.b], writes=[KCg.b])
                    else:
                        for ct in range(c0 // 128, (c0 + n + 127) // 128):
                            m = min(128, NC - ct * 128)
                            lo = ct * 128 - c0
                            p = nps()
                            k.mm(p.b, [(p[0:m, 0:128], hdn[:, half, lo:lo + m], w2b[:, half, :]) for half in range(2)], reads=[w2b.b, hdn.b])
                            k.op("dve", lambda e: e.tensor_tensor(out=VCOg[0:m, ct, 0:128], in0=p[0:m, 0:128], in1=b2r[0:m, :], op=ALU.add), reads=[p.b, b2r.b], writes=[VCOg.b])
            k.dma("sp", KSg[:, :], kvfm[(4 + g) * 128:(5 + g) * 128, :], writes=[KSg.b])
            for t16 in range(0, NKT, 16):
                k.dma("sp", VS[:, t16:t16 + 16, 0:128], vtm[t16 * 128:(t16 + 16) * 128, g * 128:(g + 1) * 128].rearrange("(t p) c -> p t c", p=128), writes=[VS.b])
            for i in range(NQ):
                KTN = 4 * i + 4
                k.dma("sp", gt[:, :], gts[i * 128:(i + 1) * 128, :], writes=[gt.b])
                k.dma("sp", qg[:, :, :], qT[g * 1024:(g + 1) * 1024, i * 128:(i + 1) * 128].rearrange("(h d) q -> d h q", d=128), writes=[qg.b])
                c_hi = min(32 * i + 31, NC)
                nct = (c_hi + 127) // 128
                c_part = 32 * i - 1
                for ct in range(nct):
                    m = min(128, c_hi - ct * 128)
                    for hf in range(2):
                        p = nps()
                        mms = [(p[0:m, 0:512], KCg[:, ct * 128:ct * 128 + m], qg[:, hf * 4:(hf + 1) * 4, :].rearrange("p h q -> p (h q)"))]
                        rb_ = [KCg.b, qg.b]
                        if ct * 128 + m > c_part:
                            m0 = c_part - ct * 128
                            mms.append((p[0:m, 0:512], zc[:, 160 - m0:160 - m0 + m], bc[:, :]))
                            rb_ += [zc.b, bc.b]
                        k.mm(p.b, mms, reads=rb_)
                        k.op("act", lambda e: e.activation(out=ET[0:m, ct, hf * 512:(hf + 1) * 512], in_=p[0:m, 0:512], func=AF.Exp), reads=[p.b], writes=[ET.b])
                for hh in range(8):
                    p = nps()
                    mms = []
                    for ct in range(nct):
                        m = min(128, c_hi - ct * 128)
                        mms.append((p[:, 0:W], ET[0:m, ct, hh * 128:(hh + 1) * 128], VCOg[0:m, ct, :]))
                    k.mm(p.b, mms, reads=[ET.b, VCOg.b])
                    finish_head(p[:, 0:W], p.b, 0, g, hh, True)
                off = NS - 8 * i
                k.op("dve", lambda e: e.tensor_tensor(out=imp[:, :], in0=imp[:, :], in1=pb[:, off:off + NS], op=ALU.add), reads=[imp.b, pb.b], writes=[imp.b])
                k.op("dve", lambda e: e.tensor_scalar(out=imp[:, 0:1], in0=imp[:, 0:1], scalar1=1e4, scalar2=None, op0=ALU.add), reads=[imp.b], writes=[imp.b])
                k.op("dve", lambda e: e.max(out=m8a[:, :], in_=imp[:, :]), reads=[imp.b], writes=[m8a.b])
                k.op("dve", lambda e: e.match_replace(out=sc2[:, :], in_to_replace=m8a[:, :], in_values=imp[:, :], imm_value=-1e9), reads=[imp.b, m8a.b], writes=[sc2.b])
                k.op("dve", lambda e: e.max(out=m8b[:, :], in_=sc2[:, :]), reads=[sc2.b], writes=[m8b.b])
                k.op("dve", lambda e: e.tensor_scalar(out=selb[:, :], in0=imp[:, :], scalar1=m8b[:, 7:8], scalar2=NEG, op0=ALU.is_lt, op1=ALU.mult), reads=[imp.b, m8b.b], writes=[selb.b])
                for sc in range(NSC):
                    p = nps()
                    k.pe_raw(lambda: nc.tensor.transpose(p[0:SW, 0:128], selb[:, sc * 128:sc * 128 + SW], cf[:, :]), reads=[selb.b, cf.b], writes=[p.b])
                    k.op("dve", lambda e: e.tensor_copy(out=selT4[0:SW, sc, :, :], in_=p[0:SW, 0:128].unsqueeze(1).to_broadcast([SW, 4, 128])), reads=[p.b], writes=[selT4.b])
                tiles = []
                for kt in range(KTN):
                    sc = (2 * kt) // 128
                    pat = kt % 64
                    masks = [(cb[0:SW, 1152 + pat * 128:1152 + (pat + 1) * 128], selT4[0:SW, sc, :, :].rearrange("p h q -> p (h q)"), [cb.b, selT4.b])]
                    r = kt - 4 * i
                    if r >= 0:
                        masks.append((identb, dm[:, r * 512:(r + 1) * 512], [cb.b, dm.b]))
                    tiles.append((KSg[:, kt * 128:(kt + 1) * 128], [KSg.b], VS[:, kt, :], [VS.b], masks))
                dense_branch(1, g, tiles)
                kt0 = 4 * i - 4
                lo = max(kt0, 0)
                nw = 4 * i + 4 - lo
                k.dma("sp", kwt[:, 0:nw, :], kvfm[(6 + g) * 128:(7 + g) * 128, lo * 128:(lo + nw) * 128].rearrange("d (t k) -> d t k", k=128), writes=[kwt.b])
                k.dma("sp", vwt[:, 0:nw, 0:128], vtm[lo * 128:(lo + nw) * 128, 256 + g * 128:256 + (g + 1) * 128].rearrange("(t p) c -> p t c", p=128), writes=[vwt.b])
                tiles = []
                for w_ in range(nw):
                    r = lo + w_ - kt0
                    tiles.append((kwt[:, w_, :], [kwt.b], vwt[:, w_, :], [vwt.b], [(identb, wm[:, r * 512:(r + 1) * 512], [cb.b, wm.b])]))
                dense_branch(2, g, tiles)
                if DBG:
                    k.dma("sp", dbg_o[i * 128:(i + 1) * 128, g * 1024:(g + 1) * 1024], O[:, :], reads=[O.b], writes=[Bo])
                for q4 in range(2):
                    p = nps()

                    def tr():
                        for x in range(4):
                            c = q4 * 4 + x
                            ins = nc.tensor.transpose(p[:, x * 128:(x + 1) * 128], O[:, c * 128:(c + 1) * 128], cf[:, :])
                        return ins
                    k.pe_raw(tr, reads=[O.b, cf.b], writes=[p.b])
                    k.op("act", lambda e: e.copy(out=oT[:, q4 * 4:(q4 + 1) * 4, :], in_=p[:, :].rearrange("p (a t) -> p a t", a=4)), reads=[p.b], writes=[oT.b])
                k.dma("sp", d["mixT"].rearrange("(c p) t -> p c t", p=128)[:, g * 8:(g + 1) * 8, i * 128:(i + 1) * 128], oT[:, :, :], reads=[oT.b], writes=[d["mixT_buf"]])
        barrier(k)
    sb2, ps2 = mk(nc)
    P2 = [ps2("Q%d" % i, [128, 512]) for i in range(8)]
    pj = [0]

    def nps2():
        pj[0] = (pj[0] + 1) % 8
        return P2[pj[0]]

    emit_post_moe(nc, k, sb2, ps2, P2, nps2, TL, 0, d, None, None, mode="p1")
    return nc


NG = 4


def _router(inp, layer):
    rw, rb = inp["moe_router_w"][layer], inp["moe_router_b"][layer]
    if rw.shape[1] < 32:
        pad = 32 - rw.shape[1]
        rw = np.concatenate([rw, np.zeros((rw.shape[0], pad), np.float32)], 1)
        rb = np.concatenate([rb, np.full((pad,), -1e4, np.float32)])
    return {"rw": np.ascontiguousarray(rw), "rb": np.ascontiguousarray(rb[None, :])}


def _run(nc, maps):
    res = run_bass_kernel_spmd(nc, maps, core_ids=list(range(8)))
    return [{k_: np.asarray(v) for k_, v in res.results[c].items()} for c in range(8)]


def _post_layer(inp, layer, TL, E, p1out, cst, nsa_maps):
    EG = E // NG
    acc = None
    for gi in range(NG):
        es = slice(gi * EG, (gi + 1) * EG)
        nc = build_MO(TL, EG, gi == 0)
        shared = {"wgu": inp["moe_w_gu"][layer, es], "bgu": np.ascontiguousarray(inp["moe_b_gu"][layer, es].reshape(EG, 32, 128).transpose(0, 2, 1)),
                  "wd": inp["moe_w_down"][layer, es], "bd": np.ascontiguousarray(inp["moe_b_down"][layer, es]), "cst": cst}
        maps = []
        for c in range(8):
            m = {"x1T": p1out[c]["x1T"], "gts": np.ascontiguousarray(p1out[c]["gates"][:, es])}
            if gi:
                m["accin"] = acc[c]["acc"]
            m.update(shared)
            maps.append(m)
        acc = _run(nc, maps)
    nc = build_P3(TL, nsa_maps is not None)
    lnv = np.ascontiguousarray(np.stack([inp["ln_g"][layer, 1], inp["ln_b"][layer, 1]]))
    maps = []
    for c in range(8):
        m = {"lnv": lnv, "cst": cst, "x1": p1out[c]["x1"], "accin": acc[c]["acc"]}
        if nsa_maps is not None:
            m.update(nsa_maps)
        maps.append(m)
    return _run(nc, maps)


def kernel(**inputs):
    inp = {k_: np.asarray(v) for k_, v in inputs.items()}
    Bn, T, D = inp["x"].shape
    TL = T // 4
    E = inp["moe_w_gu"].shape[1]
    cores = list(range(8))
    bf = ml_dtypes.bfloat16
    cst = consts_A()
    idx = [np.concatenate([np.arange((4 * i + j) * 128, (4 * i + j + 1) * 128) for i in range(TL // 128)]) for j in range(4)]
    yg = [o["yg"] for o in _run(build_A(T), [inputs_A(inp, T, c) for c in cores])]
    lnv0 = np.ascontiguousarray(np.stack([inp["ln_g"][0, 0], inp["ln_b"][0, 0]]))
    maps = []
    for c in cores:
        b, j = c // 4, c % 4
        ygb = np.concatenate([yg[b * 4 + jj][idx[j]] for jj in range(4)], axis=1)
        maps.append({"mixT": np.ascontiguousarray(ygb.T), "xres": np.ascontiguousarray(inp["x"][b][idx[j]]), "wo": inp["rw_w_o"][0], "cst": cst, "lnv": lnv0, **_router(inp, 0)})
    p1 = _run(build_P1(TL), maps)
    del yg, maps
    outB = _post_layer(inp, 0, TL, E, p1, cst, {"wkv": inp["nsa_w_kv"], "win": inp["nsa_w_in"][0], "bgate": np.ascontiguousarray(inp["nsa_b_gate"][0][None, :])})
    csC = consts_C(T)
    kv_all = []
    for b in range(Bn):
        kvfm = np.zeros((1024, T), bf)
        vtm = np.zeros((T, 512), bf)
        for j in range(4):
            kvfm[:, idx[j]] = outB[b * 4 + j]["kvfm"]
            vtm[idx[j]] = outB[b * 4 + j]["kvtm"]
        kv_all.append((kvfm, vtm))
    lnv1 = np.ascontiguousarray(np.stack([inp["ln_g"][1, 0], inp["ln_b"][1, 0]]))
    maps = []
    for c in cores:
        b, j = c // 4, c % 4
        m = {"kvfm": kv_all[b][0], "vtm": kv_all[b][1], "qT": outB[c]["qT"], "gts": outB[c]["gts"], "xres": outB[c]["x2"],
             "peT": np.ascontiguousarray(inp["nsa_cmp_pe"].transpose(0, 2, 1)), "cw1": inp["nsa_cmp_w1"],
             "cb1": np.ascontiguousarray(inp["nsa_cmp_b1"].reshape(2, 2, 128).transpose(0, 2, 1)), "cw2": inp["nsa_cmp_w2"], "cb2": inp["nsa_cmp_b2"],
             "cst": cst, "wo": inp["nsa_w_o"][0], "lnv": lnv1, **_router(inp, 1)}
        m.update(csC[j])
        maps.append(m)
    p1 = _run(build_C(T), maps)
    del outB, maps, kv_all
    fin = _post_layer(inp, 1, TL, E, p1, cst, None)
    out = np.zeros((Bn, T, D), np.float32)
    for c in cores:
        b, j = c // 4, c % 4
        out[b, idx[j]] = fin[c]["x2"]
    return out
```
